# Optimizing a Trainium2 kernel written in Bass

```python
import jax
import jax.numpy as jnp
from jax import lax
import numpy as np

D_MODEL = 1024
BATCH = 4
SEQ = 4096
DEPTH = 4

GRID_W = 64
MEM_LEN = 256
HEAD_DIM = 64
Q_BLOCK = 128
ROPE_THETA = 10000.0
LN_EPS = 1e-5
RMS_EPS = 1e-6
NA_HEADS = 4
NA_ROWS = 8
NA_COLS = 16
GQA_HEADS = 8
GQA_KV_HEADS = 2
GQA_GROUP = GQA_HEADS // GQA_KV_HEADS
MLA_HEADS = 4
MLA_Q_RANK = 256
MLA_KV_RANK = 128
MLA_NOPE = 64
MLA_ROPE = 32
MLA_V = 64
N_BRANCH = 3
A_WIDTH = NA_HEADS * HEAD_DIM
B_WIDTH = GQA_HEADS * HEAD_DIM
C_WIDTH = MLA_HEADS * MLA_V
IN_SPLITS = (A_WIDTH, A_WIDTH, A_WIDTH,
             B_WIDTH, GQA_KV_HEADS * HEAD_DIM, GQA_KV_HEADS * HEAD_DIM,
             MLA_Q_RANK, MLA_KV_RANK, MLA_ROPE,
             N_BRANCH * D_MODEL)
IN_COLS = sum(IN_SPLITS)
MEM_HEADS = 4
MEM_HEAD_DIM = D_MODEL // MEM_HEADS
D_FF = ((8 * D_MODEL // 3 + 127) // 128) * 128
N_EXPERTS = 8
TOP_K = 2
N_DENSE = (DEPTH + 1) // 2
N_MOE = DEPTH // 2
ALPHA = (2 * DEPTH) ** 0.25
BETA = (8 * DEPTH) ** -0.25

kernel_name = 'hybrid_natten_gqa_mla_moe_encoder'


def layer_norm(x, g, b):
    xf = x.astype(jnp.float32)
    mu = jnp.mean(xf, axis=-1, keepdims=True)
    var = jnp.mean(jnp.square(xf - mu), axis=-1, keepdims=True)
    return ((xf - mu) * lax.rsqrt(var + LN_EPS) * g + b).astype(x.dtype)


def rms_norm(x, g):
    xf = x.astype(jnp.float32)
    return (xf * lax.rsqrt(jnp.mean(xf * xf, axis=-1, keepdims=True) + RMS_EPS) * g).astype(x.dtype)


def axial_rope_angles(seq, rot_dim):
    t = jnp.arange(seq)
    row = (t // GRID_W).astype(jnp.float32)
    col = (t % GRID_W).astype(jnp.float32)
    n = rot_dim // 4
    inv_freq = ROPE_THETA ** (-jnp.arange(n, dtype=jnp.float32) / n)
    ang = jnp.concatenate([row[:, None] * inv_freq, col[:, None] * inv_freq], axis=-1)
    return jnp.cos(ang), jnp.sin(ang)


def apply_rope(x, cos, sin):
    half = x.shape[-1] // 2
    xf = x.astype(jnp.float32)
    x1, x2 = xf[..., :half], xf[..., half:]
    c, s = cos[None, :, None, :], sin[None, :, None, :]
    return jnp.concatenate([x1 * c - x2 * s, x2 * c + x1 * s], axis=-1).astype(x.dtype)


def blocked_attention(q, k, v, scale):
    b, s, hkv, g, dk = q.shape
    nb = s // Q_BLOCK
    qb = q.reshape(b, nb, Q_BLOCK, hkv, g, dk).swapaxes(0, 1)

    def attend(q_blk):
        sc = jnp.einsum('bqhgd,bkhd->bhgqk', q_blk, k).astype(jnp.float32) * scale
        p = jax.nn.softmax(sc, axis=-1).astype(v.dtype)
        return jnp.einsum('bhgqk,bkhd->bqhgd', p, v)

    o = lax.map(attend, qb)
    return o.swapaxes(0, 1).reshape(b, s, hkv * g * v.shape[-1])


def neighborhood_attention(q, k, v, rpb):
    b, s, h, d = q.shape
    rows = s // GRID_W
    kr = min(NA_ROWS, rows)
    n_keys = kr * NA_COLS
    r = jnp.arange(rows)
    c = jnp.arange(GRID_W)
    key_r = jnp.clip(r - kr // 2, 0, rows - kr)[:, None] + jnp.arange(kr)
    key_c = jnp.clip(c - NA_COLS // 2, 0, GRID_W - NA_COLS)[:, None] + jnp.arange(NA_COLS)
    idx = (key_r[:, None, :, None] * GRID_W + key_c[None, :, None, :]).reshape(rows, GRID_W, n_keys)
    dr = key_r - r[:, None] + (NA_ROWS - 1)
    dc = key_c - c[:, None] + (NA_COLS - 1)
    bias = rpb[:, dr[:, None, :, None], dc[None, :, None, :]]
    bias = bias.reshape(h, rows, GRID_W, n_keys).transpose(1, 0, 2, 3)
    q_rows = q.reshape(b, rows, GRID_W, h, d).swapaxes(0, 1)
    scale = d ** -0.5

    def attend_row(args):
        q_r, idx_r, bias_r = args
        k_g = k[:, idx_r]
        v_g = v[:, idx_r]
        sc = jnp.einsum('bwhd,bwnhd->bhwn', q_r, k_g).astype(jnp.float32) * scale + bias_r.astype(jnp.float32)
        p = jax.nn.softmax(sc, axis=-1).astype(v.dtype)
        return jnp.einsum('bhwn,bwnhd->bwhd', p, v_g)

    o = lax.map(attend_row, (q_rows, idx, bias))
    return o.swapaxes(0, 1).reshape(b, s, h * d)


def hybrid_mixer(h, w_in, na_rpb, gqa_q_norm, gqa_k_norm, mla_q_norm, mla_kv_norm,
                 mla_w_uq, mla_w_ukv, w_proj_a, w_proj_b, w_proj_c, w_out, rope_b, rope_c):
    b, s, _ = h.shape
    z = h @ w_in
    (a_q, a_k, a_v, b_q, b_k, b_v, c_dq, c_dkv, c_kr, gate_logits) = jnp.split(
        z, np.cumsum(IN_SPLITS)[:-1].tolist(), axis=-1)

    ya = neighborhood_attention(a_q.reshape(b, s, NA_HEADS, HEAD_DIM),
                                a_k.reshape(b, s, NA_HEADS, HEAD_DIM),
                                a_v.reshape(b, s, NA_HEADS, HEAD_DIM), na_rpb)

    cos_b, sin_b = rope_b
    qb = apply_rope(rms_norm(b_q.reshape(b, s, GQA_HEADS, HEAD_DIM), gqa_q_norm), cos_b, sin_b)
    kb = apply_rope(rms_norm(b_k.reshape(b, s, GQA_KV_HEADS, HEAD_DIM), gqa_k_norm), cos_b, sin_b)
    vb = b_v.reshape(b, s, GQA_KV_HEADS, HEAD_DIM)
    yb = blocked_attention(qb.reshape(b, s, GQA_KV_HEADS, GQA_GROUP, HEAD_DIM), kb, vb, HEAD_DIM ** -0.5)

    cos_c, sin_c = rope_c
    qc = (rms_norm(c_dq, mla_q_norm) @ mla_w_uq).reshape(b, s, MLA_HEADS, MLA_NOPE + MLA_ROPE)
    q_nope, q_pe = qc[..., :MLA_NOPE], apply_rope(qc[..., MLA_NOPE:], cos_c, sin_c)
    kvc = (rms_norm(c_dkv, mla_kv_norm) @ mla_w_ukv).reshape(b, s, MLA_HEADS, MLA_NOPE + MLA_V)
    k_nope, vc = kvc[..., :MLA_NOPE], kvc[..., MLA_NOPE:]
    k_pe = apply_rope(c_kr[:, :, None, :], cos_c, sin_c)
    q_full = jnp.concatenate([q_nope, q_pe], axis=-1)[:, :, :, None, :]
    k_full = jnp.concatenate([k_nope, jnp.broadcast_to(k_pe, (b, s, MLA_HEADS, MLA_ROPE))], axis=-1)
    yc = blocked_attention(q_full, k_full, vc, (MLA_NOPE + MLA_ROPE) ** -0.5)

    gates = jax.nn.sigmoid(gate_logits.astype(jnp.float32)).astype(h.dtype).reshape(b, s, N_BRANCH, D_MODEL)
    merged = (gates[:, :, 0] * (ya @ w_proj_a)
              + gates[:, :, 1] * (yb @ w_proj_b)
              + gates[:, :, 2] * (yc @ w_proj_c))
    return merged @ w_out


def memory_cross_attention(h, mem, wq, wkv, wo):
    b, s, _ = h.shape
    m = mem.shape[1]
    q = (h @ wq).reshape(b, s, MEM_HEADS, MEM_HEAD_DIM)
    kv = (mem @ wkv).reshape(b, m, 2, MEM_HEADS, MEM_HEAD_DIM)
    k, v = kv[:, :, 0], kv[:, :, 1]
    sc = jnp.einsum('bqhd,bkhd->bhqk', q, k).astype(jnp.float32) * (MEM_HEAD_DIM ** -0.5)
    p = jax.nn.softmax(sc, axis=-1).astype(v.dtype)
    o = jnp.einsum('bhqk,bkhd->bqhd', p, v).reshape(b, s, D_MODEL)
    return o @ wo


def swiglu(t, w_gu, w_down):
    a, g = jnp.split(t @ w_gu, 2, axis=-1)
    return (jax.nn.silu(a) * g) @ w_down


def moe_swiglu(h, w_router, w_gu, w_down):
    b, s, d = h.shape
    t = h.reshape(b * s, d)
    logits = (t @ w_router).astype(jnp.float32)
    top_logit, top_idx = lax.top_k(logits, TOP_K)
    top_w = jax.nn.softmax(top_logit, axis=-1)
    gate = jnp.einsum('tk,tke->te', top_w,
                      jax.nn.one_hot(top_idx, N_EXPERTS, dtype=jnp.float32)).astype(h.dtype)
    out = jnp.zeros_like(t)
    for e in range(N_EXPERTS):
        out = out + gate[:, e:e + 1] * swiglu(t, w_gu[e], w_down[e])
    return out.reshape(b, s, d)


def setup_inputs(seed: int = 0) -> dict:
    key = jax.random.key(seed)
    k = jax.random.split(key, 26)

    def nrm(kk, shape, std):
        return jax.random.normal(kk, shape, jnp.float32) * std

    def gain(kk, shape):
        return 1.0 + 0.05 * jax.random.normal(kk, shape, jnp.float32)

    return {
        'x': nrm(k[0], (BATCH, SEQ, D_MODEL), 1.0),
        'mem': nrm(k[1], (BATCH, MEM_LEN, D_MODEL), 1.0),
        'emb_ln_g': gain(k[2], (D_MODEL,)),
        'emb_ln_b': nrm(k[3], (D_MODEL,), 0.02),
        'w_in': nrm(k[4], (DEPTH, D_MODEL, IN_COLS), D_MODEL ** -0.5),
        'na_rpb': nrm(k[5], (DEPTH, NA_HEADS, 2 * NA_ROWS - 1, 2 * NA_COLS - 1), 0.1),
        'gqa_q_norm': gain(k[6], (DEPTH, HEAD_DIM)),
        'gqa_k_norm': gain(k[7], (DEPTH, HEAD_DIM)),
        'mla_q_norm': gain(k[8], (DEPTH, MLA_Q_RANK)),
        'mla_kv_norm': gain(k[9], (DEPTH, MLA_KV_RANK)),
        'mla_w_uq': nrm(k[10], (DEPTH, MLA_Q_RANK, MLA_HEADS * (MLA_NOPE + MLA_ROPE)), MLA_Q_RANK ** -0.5),
        'mla_w_ukv': nrm(k[11], (DEPTH, MLA_KV_RANK, MLA_HEADS * (MLA_NOPE + MLA_V)), MLA_KV_RANK ** -0.5),
        'w_proj_a': nrm(k[12], (DEPTH, A_WIDTH, D_MODEL), A_WIDTH ** -0.5),
        'w_proj_b': nrm(k[13], (DEPTH, B_WIDTH, D_MODEL), B_WIDTH ** -0.5),
        'w_proj_c': nrm(k[14], (DEPTH, C_WIDTH, D_MODEL), C_WIDTH ** -0.5),
        'w_out': nrm(k[15], (DEPTH, D_MODEL, D_MODEL), BETA * D_MODEL ** -0.5),
        'mem_wq': nrm(k[16], (DEPTH, D_MODEL, D_MODEL), D_MODEL ** -0.5),
        'mem_wkv': nrm(k[17], (DEPTH, D_MODEL, 2 * D_MODEL), D_MODEL ** -0.5),
        'mem_wo': nrm(k[18], (DEPTH, D_MODEL, D_MODEL), BETA * D_MODEL ** -0.5),
        'ln_g': gain(k[19], (DEPTH, 3, D_MODEL)),
        'ln_b': nrm(k[20], (DEPTH, 3, D_MODEL), 0.02),
        'ffn_w_gu': nrm(k[21], (N_DENSE, D_MODEL, 2 * D_FF), D_MODEL ** -0.5),
        'ffn_w_down': nrm(k[22], (N_DENSE, D_FF, D_MODEL), BETA * D_FF ** -0.5),
        'moe_router': nrm(k[23], (N_MOE, D_MODEL, N_EXPERTS), D_MODEL ** -0.5),
        'moe_w_gu': nrm(k[24], (N_MOE, N_EXPERTS, D_MODEL, 2 * D_FF), D_MODEL ** -0.5),
        'moe_w_down': nrm(k[25], (N_MOE, N_EXPERTS, D_FF, D_MODEL), BETA * D_FF ** -0.5),
    }


def reference(x, mem, emb_ln_g, emb_ln_b, w_in, na_rpb, gqa_q_norm, gqa_k_norm, mla_q_norm,
              mla_kv_norm, mla_w_uq, mla_w_ukv, w_proj_a, w_proj_b, w_proj_c, w_out,
              mem_wq, mem_wkv, mem_wo, ln_g, ln_b, ffn_w_gu, ffn_w_down,
              moe_router, moe_w_gu, moe_w_down):
    s = x.shape[1]
    rope_b = axial_rope_angles(s, HEAD_DIM)
    rope_c = axial_rope_angles(s, MLA_ROPE)
    h = layer_norm(x, emb_ln_g, emb_ln_b)
    for l in range(DEPTH):
        mix = hybrid_mixer(h, w_in[l], na_rpb[l], gqa_q_norm[l], gqa_k_norm[l], mla_q_norm[l],
                           mla_kv_norm[l], mla_w_uq[l], mla_w_ukv[l], w_proj_a[l], w_proj_b[l],
                           w_proj_c[l], w_out[l], rope_b, rope_c)
        h = layer_norm(ALPHA * h + mix, ln_g[l, 0], ln_b[l, 0])
        xa = memory_cross_attention(h, mem, mem_wq[l], mem_wkv[l], mem_wo[l])
        h = layer_norm(ALPHA * h + xa, ln_g[l, 1], ln_b[l, 1])
        if l % 2 == 0:
            f = swiglu(h, ffn_w_gu[l // 2], ffn_w_down[l // 2])
        else:
            f = moe_swiglu(h, moe_router[l // 2], moe_w_gu[l // 2], moe_w_down[l // 2])
        h = layer_norm(ALPHA * h + f, ln_g[l, 2], ln_b[l, 2])
    return h
```

```python
import numpy as np
import concourse.bass as bass
import concourse.mybir as mybir
from concourse.bass_utils import run_bass_kernel_spmd

F32 = mybir.dt.float32
BF16 = mybir.dt.bfloat16
AF = mybir.ActivationFunctionType
ALU = mybir.AluOpType
AX = mybir.AxisListType

D = 1024
S = 4096
DEPTH = 4
INC = 5024
DFF = 2816
NE = 8
ALPHA = float((2 * DEPTH) ** 0.25)
LN_EPS = 1e-5
RMS_EPS = 1e-6
NEG = -30000.0


class Buf:
    __slots__ = ("name", "w", "r", "prev", "x")

    def __init__(self, name="", x=False):
        self.name = name
        self.x = x
        self.w = {}
        self.r = {}
        self.prev = {}


def _merge(d, s):
    for k, v in s.items():
        if d.get(k, 0) < v:
            d[k] = v


class FW:
    def __init__(self, nc, n_main=40, n_cv=16):
        self.nc = nc
        self._cms = []
        self._scopes = []
        self.eng = {"pe": nc.tensor, "act": nc.scalar, "dve": nc.vector, "pool": nc.gpsimd, "sp": nc.sync}
        self.sems = {}
        self.count = {}
        self.known = {e: {} for e in self.eng}
        for e in ("pe", "act", "dve", "pool"):
            self.sems[e] = self._sem("tl_" + e)
            self.count[e] = 0
        self.pools = {"main": [], "cv": []}
        self.rr = {"main": 0, "cv": 0}
        for pn, n in (("main", n_main), ("cv", n_cv)):
            for i in range(n):
                k = "%s%d" % (pn, i)
                self.sems[k] = self._sem(k)
                self.count[k] = 0
                self.pools[pn].append(k)
        self.n_inst = 0
        self.n_wait = 0

    def _sem(self, name):
        cm = self.nc.semaphore(name)
        s = cm.__enter__()
        self._cms.append(cm)
        return s

    def push(self):
        self._scopes.append([])

    def pop(self):
        for cm in reversed(self._scopes.pop()):
            cm.__exit__(None, None, None)

    def _alloc(self, cm):
        t = cm.__enter__()
        (self._scopes[-1] if self._scopes else self._cms).append(cm)
        return t

    def sb(self, name, shape, dt):
        self._uid = getattr(self, "_uid", 0) + 1
        return self._alloc(self.nc.sbuf_tensor("%s_u%d" % (name, self._uid), shape, dt))

    def ps(self, name, shape, dt):
        return self._alloc(self.nc.psum_tensor(name, shape, dt))

    def close(self):
        for cm in reversed(self._cms):
            cm.__exit__(None, None, None)

    def _wait(self, e, deps):
        kn = self.known[e]
        eng = self.eng[e]
        for k, v in deps.items():
            if kn.get(k, 0) >= v:
                continue
            eng.wait_ge(self.sems[k], v)
            kn[k] = v
            self.n_wait += 1

    def _deps(self, reads, writes, joins):
        deps = {}
        for b in reads:
            _merge(deps, b.w)
            if b.x:
                _merge(deps, b.r)
        for b in writes:
            _merge(deps, b.w)
            _merge(deps, b.r)
        for b in joins:
            _merge(deps, b.prev)
            _merge(deps, b.r)
        return deps

    def _commit(self, ev, reads, writes, joins):
        k, v = ev
        for b in writes:
            p = {}
            _merge(p, b.w)
            _merge(p, b.r)
            b.prev = p
            b.w = {k: v}
            b.r = {}
        for b in joins:
            if b.w.get(k, 0) < v:
                b.w[k] = v
        for b in reads:
            if b.r.get(k, 0) < v:
                b.r[k] = v

    def op(self, e, fn, reads=(), writes=(), joins=()):
        deps = self._deps(reads, writes, joins)
        if e == "pe":
            deps.pop("pe", None)
        self._wait(e, deps)
        ins = fn()
        self.count[e] += 1
        ins.then_inc(self.sems[e], 1)
        self._commit((e, self.count[e]), reads, writes, joins)
        self.n_inst += 1
        return ins

    def dma(self, q, out, in_, reads=(), writes=(), joins=(), pool="main", **kw):
        pl = self.pools[pool]
        k = pl[self.rr[pool]]
        self.rr[pool] = (self.rr[pool] + 1) % len(pl)
        deps = self._deps(reads, writes, joins)
        if self.count[k] > 0:
            _merge(deps, {k: self.count[k]})
        self._wait(q, deps)
        ins = self.eng[q].dma_start(out=out, in_=in_, **kw)
        self.count[k] += 16
        ins.then_inc(self.sems[k], 16)
        self._commit((k, self.count[k]), reads, writes, joins)
        self.n_inst += 1
        return ins

    def barrier(self):
        deps = {}
        for e in ("pe", "act", "dve"):
            if self.count[e]:
                deps[e] = self.count[e]
        for k in self.pools["main"]:
            if self.count[k]:
                deps[k] = self.count[k]
        for e in ("pe", "act", "dve", "sp"):
            d = dict(deps)
            d.pop(e, None)
            self._wait(e, d)


class Prog:
    def __init__(self, nlayers=DEPTH, T=S, debug=False, mini=False):
        self.nl = nlayers
        self.T = T
        self.NT = T // 128
        self.debug = debug
        nc = self.nc = bass.Bass("TRN2", target_bir_lowering=False)
        self.f = FW(nc)
        self.used_inputs = []

        def ei(n, s):
            big = n not in ("x", "mem", "emb_ln_g", "emb_ln_b")
            skip = (mini and big) or (nlayers < 2 and n.startswith("moe_"))
            self.used_inputs.append(n) if not skip else None
            if skip:
                return None
            return nc.dram_tensor(n, s, F32, kind="ExternalInput").ap()
        self.x = ei("x", [T, D])
        self.mem = ei("mem", [256, D])
        self.emb_g = ei("emb_ln_g", [1, D])
        self.emb_b = ei("emb_ln_b", [1, D])
        self.w_in = ei("w_in", [DEPTH, D, INC])
        self.nab = ei("nab", [DEPTH, 5, 5, 128, 512])
        self.gqn = ei("gqa_q_norm", [DEPTH, 64])
        self.gkn = ei("gqa_k_norm", [DEPTH, 64])
        self.mqn = ei("mla_q_norm", [DEPTH, 256])
        self.mkn = ei("mla_kv_norm", [DEPTH, 128])
        self.wuq = ei("mla_w_uq", [DEPTH, 256, 384])
        self.wukv = ei("mla_w_ukv", [DEPTH, 128, 512])
        self.wpa = ei("w_proj_a", [DEPTH, 256, D])
        self.wpb = ei("w_proj_b", [DEPTH, 512, D])
        self.wpc = ei("w_proj_c", [DEPTH, 256, D])
        self.wout = ei("w_out", [DEPTH, D, D])
        self.mwq = ei("mem_wq", [DEPTH, D, D])
        self.mwkv = ei("mem_wkv", [DEPTH, D, 2 * D])
        self.mwo = ei("mem_wo", [DEPTH, D, D])
        self.ln_g = ei("ln_g", [DEPTH, 3, D])
        self.ln_b = ei("ln_b", [DEPTH, 3, D])
        self.fgu = ei("ffn_w_gu", [2, D, 2 * DFF])
        self.fdn = ei("ffn_w_down", [2, DFF, D])
        self.mrt = ei("moe_router_t", [2, NE, D])
        self.mgu = ei("moe_w_gu", [2, NE, D, 2 * DFF])
        self.mdn = ei("moe_w_down", [2, NE, DFF, D])
        self.rope = ei("rope", [S, 96])
        self.out = nc.dram_tensor("out", [T, D], F32, kind="ExternalOutput").ap()
        it = lambda n, s, d=BF16: nc.dram_tensor(n, s, d, kind="Internal").ap()
        NT = self.NT
        self.H = it("H", [T, D], F32)
        self.HT = it("HT", [NT, 128, 8, 128])
        self.MEMT = it("MEMT", [128, 8, 256])
        self.AQT = it("AQT", [2, 128, S]); self.AKT = it("AKT", [2, 128, S])
        self.AV1 = it("AV1", [S, 4, 65])
        self.BQT = it("BQT", [4, 128, S]); self.BKT = it("BKT", [2, 128, S])
        self.BV1 = it("BV1", [S, 2, 65])
        self.CQT = it("CQT", [4, 96, S]); self.CKT = it("CKT", [4, 96, S])
        self.CV1 = it("CV1", [S, 4, 65])
        self.GT = it("GT", [24, 128, T])
        self.YT = it("YT", [16, 64, T])
        self.GATE = it("GATE", [T, NE], F32)
        self.bHt = [Buf("H%d" % i) for i in range(NT)]; self.bHTt = [Buf("HT%d" % i) for i in range(NT)]; self.bMEMT = Buf("MEMT")
        self.bA = Buf("attn_in"); self.bGT = Buf("GT"); self.bYT = Buf("YT"); self.bGATE = Buf("GATE")
        L = nlayers
        self.WINFM = it("WINFM", [L, 31, 128, 8, 128]); self.WINTM = it("WINTM", [L, 128, 8, 1440])
        self.WPA = it("WPA", [L, 128, 2, D]); self.WPB = it("WPB", [L, 128, 4, D]); self.WPC = it("WPC", [L, 128, 2, D])
        self.WOUT = it("WOUT", [L, 128, 8, D]); self.WQ = it("WQ", [L, 128, 8, D]); self.WKV = it("WKV", [L, 128, 8, 2 * D])
        self.WO = it("WO", [L, 128, 8, D])
        nE = sum(1 if l % 2 == 0 else NE for l in range(L))
        self.WGU = it("WGU", [nE, 22, 128, 8, 256]); self.WD = it("WD", [nE, 128, 22, D])
        self.bW = {}
        self.ebase = {}
        b = 0
        for l in range(L):
            self.ebase[l] = b
            b += 1 if l % 2 == 0 else NE
        self.dbg = {}

    def cv(self, key, out, in_):
        f = self.f
        b = self.bW.get(key)
        if b is None:
            b = self.bW[key] = Buf(key)
            f.dma("pool", out, in_, writes=[b], pool="cv")
        else:
            f.dma("pool", out, in_, joins=[b], pool="cv")

    def convert_layer_attn(self, l):
        w = self.w_in[l].rearrange("(kc p) c -> p kc c", p=128)
        fm_cols = [0, 128, 256, 384, 1536, 1664, 1792] + [1952 + 128 * i for i in range(24)]
        self.cv(("wintm", l), self.WINTM[l], w[:, :, 512:1952])
        for m, c0 in enumerate(fm_cols):
            self.cv(("winfm", l, m), self.WINFM[l, m], w[:, :, c0:c0 + 128])
        r = lambda a: a.rearrange("(kc p) c -> p kc c", p=128)
        self.cv(("wpa", l), self.WPA[l], r(self.wpa[l]))
        self.cv(("wpb", l), self.WPB[l], r(self.wpb[l]))
        self.cv(("wpc", l), self.WPC[l], r(self.wpc[l]))
        self.cv(("wout", l), self.WOUT[l], r(self.wout[l]))
        self.cv(("wq", l), self.WQ[l], r(self.mwq[l]))
        self.cv(("wkv", l), self.WKV[l], r(self.mwkv[l]))
        self.cv(("wo", l), self.WO[l], r(self.mwo[l]))

    def convert_layer_ffn(self, l):
        ne = 1 if l % 2 == 0 else NE
        for e in range(ne):
            gu = self.fgu[l // 2] if l % 2 == 0 else self.mgu[l // 2, e]
            dn = self.fdn[l // 2] if l % 2 == 0 else self.mdn[l // 2, e]
            ei = self.ebase[l] + e
            g3 = gu.rearrange("(kc p) c -> p kc c", p=128)
            for c in range(22):
                self.cv(("wgu", ei, c), self.WGU[ei, c, :, :, 0:128], g3[:, :, c * 128:(c + 1) * 128])
                self.cv(("wgu", ei, c), self.WGU[ei, c, :, :, 128:256], g3[:, :, DFF + c * 128:DFF + (c + 1) * 128])
            self.cv(("wd", ei), self.WD[ei], dn.rearrange("(c p) n -> p c n", p=128))

    def setup_common(self):
        f, nc = self.f, self.nc
        self.pb = [f.ps("bank%d" % i, [128, 512], F32) for i in range(6)]
        self.bpb = [Buf("bank%d" % i, x=True) for i in range(6)]
        self.pT = [f.ps("bankTA", [128, 512], BF16), f.ps("bankTB", [128, 512], BF16)]
        self.bpt = [Buf("ptA", x=True), Buf("ptB", x=True)]
        self.ident = f.sb("ident", [128, 128], BF16); self.bid = Buf("ident")
        identf = f.sb("identf", [128, 128], F32); bidf = Buf()
        self.ones16 = f.sb("ones16", [128, 128], BF16); self.bones = Buf("ones")
        self.ones32 = f.sb("ones32", [128, 64], F32)
        f.op("pool", lambda: nc.gpsimd.memset(identf[:], 0.0), writes=[bidf])
        f.op("pool", lambda: nc.gpsimd.affine_select(out=identf[:], in_=identf[:], compare_op=ALU.not_equal, fill=1.0,
                                                     base=0, pattern=[[-1, 128]], channel_multiplier=1),
             reads=[bidf], writes=[bidf])
        f.op("dve", lambda: nc.vector.tensor_copy(out=self.ident[:], in_=identf[:]), reads=[bidf], writes=[self.bid])
        f.op("dve", lambda: nc.vector.memset(self.ones16[:], 1.0), writes=[self.bones])
        f.op("dve", lambda: nc.vector.memset(self.ones32[:], 1.0), writes=[self.bones])
        self.sel64 = f.sb("sel64", [128, 64], F32)
        f.op("dve", lambda: nc.vector.memset(self.sel64[:], 0.0), writes=[self.bones])
        f.op("dve", lambda: nc.vector.memset(self.sel64[64:65, :], 1.0), writes=[self.bones])
        self.epsc = f.sb("epsc", [128, 2], F32)
        f.op("dve", lambda: nc.vector.memset(self.epsc[:, 0:1], LN_EPS), writes=[self.bones])
        f.op("dve", lambda: nc.vector.memset(self.epsc[:, 1:2], RMS_EPS), writes=[self.bones])
        self.ln_h = [f.sb("ln_h%d" % i, [128, D], F32) for i in range(2)]; self.bln_h = [Buf(), Buf()]
        self.ln_t = f.sb("ln_t", [128, D], F32); self.bln_t = Buf()
        self.ln_y = [f.sb("ln_y%d" % i, [128, D], F32) for i in range(2)]; self.bln_y = [Buf(), Buf()]
        self.ln_y16 = f.sb("ln_y16", [128, D], BF16); self.bln_y16 = Buf()
        self.ln_yT = [f.sb("ln_yT%d" % i, [128, D], BF16) for i in range(2)]; self.bln_yT = [Buf(), Buf()]
        self.ln_st = f.sb("ln_st", [128, 12], F32); self.ln_mv = f.sb("ln_mv", [128, 4], F32); self.bln_s = Buf()
        self.lnG = f.sb("lnG", [128, D], F32); self.lnB = f.sb("lnB", [128, D], F32); self.blnGB = Buf()
        self.ln_i = 0

    def load_ln_params(self, g_ap, b_ap):
        f = self.f
        f.dma("sp", self.lnG[:], g_ap.partition_broadcast(128), writes=[self.blnGB])
        f.dma("sp", self.lnB[:], b_ap.partition_broadcast(128), joins=[self.blnGB])

    def ln_tile(self, gt, delta, delta_bufs, dst=None):
        f, nc = self.f, self.nc
        i = self.ln_i % 2
        self.ln_i += 1
        h, bh = self.ln_h[i], self.bln_h[i]
        rows = slice(gt * 128, (gt + 1) * 128)
        if delta is None:
            f.dma("sp", h[:], self.x[rows, :], writes=[bh])
            t = h
            bt = bh
        else:
            f.dma("sp", h[:], self.H[rows, :], reads=[self.bHt[gt]], writes=[bh])
            t, bt = self.ln_t, self.bln_t
            for hf in range(2):
                cs = slice(hf * 512, (hf + 1) * 512)
                kw = dict(writes=[bt]) if hf == 0 else dict(joins=[bt])
                f.op("dve", lambda: nc.vector.scalar_tensor_tensor(out=t[:, cs], in0=h[:, cs], scalar=ALPHA, in1=delta[hf],
                                                                    op0=ALU.mult, op1=ALU.add),
                     reads=[bh, delta_bufs[hf]], **kw)
        import os
        CUT = int(os.environ.get("CUT", "99"))
        if CUT <= 0:
            return
        st, mv, bs = self.ln_st, self.ln_mv, self.bln_s
        f.op("dve", lambda: nc.vector.bn_stats(out=st[:, 0:6], in_=t[:, 0:512]), reads=[bt], writes=[bs])
        f.op("dve", lambda: nc.vector.bn_stats(out=st[:, 6:12], in_=t[:, 512:1024]), reads=[bt], joins=[bs])
        f.op("dve", lambda: nc.vector.bn_aggr(out=mv[:, 0:2], in_=st[:].rearrange("p (a b) -> p a b", b=6)), reads=[bs], joins=[bs])
        f.op("act", lambda: nc.scalar.activation(out=mv[:, 3:4], in_=mv[:, 1:2], func=AF.Sqrt, bias=self.epsc[:, 0:1], scale=1.0), reads=[bs], joins=[bs])
        f.op("dve", lambda: nc.vector.reciprocal(out=mv[:, 2:3], in_=mv[:, 3:4]), reads=[bs], joins=[bs])
        if CUT <= 1:
            return
        y, by = self.ln_y[i], self.bln_y[i]
        f.op("dve", lambda: nc.vector.tensor_scalar(out=y[:], in0=t[:], scalar1=mv[:, 0:1], scalar2=mv[:, 2:3],
                                                    op0=ALU.subtract, op1=ALU.mult), reads=[bt, bs], writes=[by])
        f.op("dve", lambda: nc.vector.tensor_mul(out=y[:], in0=y[:], in1=self.lnG[:]), reads=[by, self.blnGB], writes=[by])
        f.op("dve", lambda: nc.vector.tensor_add(out=y[:], in0=y[:], in1=self.lnB[:]), reads=[by, self.blnGB], writes=[by])
        if dst is not None:
            f.dma("sp", dst[rows, :], y[:], reads=[by], joins=[self.bout])
            return
        if CUT <= 2:
            return
        f.dma("sp", self.H[rows, :], y[:], reads=[by], writes=[self.bHt[gt]])
        if CUT <= 3:
            return
        y16, by16 = self.ln_y16, self.bln_y16
        f.op("act", lambda: nc.scalar.copy(out=y16[:], in_=y[:]), reads=[by], writes=[by16])
        yT, byT = self.ln_yT[i], self.bln_yT[i]
        for half in range(2):
            bp = self.bpt[half]
            for j in range(4):
                c = half * 4 + j
                kw = dict(writes=[bp]) if j == 0 else dict(joins=[bp])
                f.op("pe", lambda: nc.tensor.transpose(out=self.pT[half][:, j * 128:(j + 1) * 128],
                                                       in_=y16[:, c * 128:(c + 1) * 128], identity=self.ident[:]),
                     reads=[by16, self.bid], **kw)
            kw = dict(writes=[byT]) if half == 0 else dict(joins=[byT])
            if half == 0:
                f.op("dve", lambda: nc.vector.tensor_copy(out=yT[:, 0:512], in_=self.pT[0][:]), reads=[bp], **kw)
            else:
                f.op("act", lambda: nc.scalar.copy(out=yT[:, 512:1024], in_=self.pT[1][:]), reads=[bp], **kw)
        f.dma("sp", self.HT[gt], yT[:].rearrange("p (c t) -> p c t", c=8), reads=[byT], writes=[self.bHTt[gt]])

    def new_gen(self, *bufs):
        for b in bufs:
            b.w = {}; b.r = {}; b.prev = {}

    def load_hT(self, dst, bdst, sb, ntile):
        f = self.f
        t0 = sb * ntile
        for n in range(ntile):
            kw = dict(writes=[bdst]) if n == 0 else dict(joins=[bdst])
            f.dma("sp", dst[:, :, n * 128:(n + 1) * 128], self.HT[t0 + n], reads=[self.bHTt[t0 + n]], **kw)

    def phase_embed(self):
        f, nc = self.f, self.nc
        self.load_ln_params(self.emb_g[0], self.emb_b[0])
        for gt in range(self.NT):
            self.ln_tile(gt, None, None)
        import os
        if os.environ.get("SKIP_MEM"):
            return
        f.push()
        m32 = f.sb("m32", [128, D], F32); bm32 = Buf()
        m16 = f.sb("m16", [128, D], BF16); bm16 = Buf()
        mT = f.sb("mT", [128, 8, 256], BF16); bmT = Buf()
        for mt in range(2):
            f.dma("sp", m32[:], self.mem[mt * 128:(mt + 1) * 128, :], writes=[bm32])
            f.op("act", lambda: nc.scalar.copy(out=m16[:], in_=m32[:]), reads=[bm32], writes=[bm16])
            for half in range(2):
                bp = self.bpt[half]
                for j in range(4):
                    c = half * 4 + j
                    kw = dict(writes=[bp]) if j == 0 else dict(joins=[bp])
                    f.op("pe", lambda: nc.tensor.transpose(out=self.pT[half][:, j * 128:(j + 1) * 128],
                                                           in_=m16[:, c * 128:(c + 1) * 128], identity=self.ident[:]),
                         reads=[bm16, self.bid], **kw)
                kw = dict(writes=[bmT]) if (mt == 0 and half == 0) else dict(joins=[bmT])
                f.op("dve", lambda: nc.vector.tensor_copy(
                    out=mT[:, half * 4:(half + 1) * 4, mt * 128:(mt + 1) * 128],
                    in_=self.pT[half][:].rearrange("p (c t) -> p c t", c=4)), reads=[bp], **kw)
        f.dma("sp", self.MEMT, mT[:], reads=[bmT], writes=[self.bMEMT])
        f.barrier()
        f.pop()

    def phase_A(self, l):
        f, nc = self.f, self.nc
        T = self.T
        f.push()
        wtm = f.sb("wtm", [128, 8, 1440], BF16); bwtm = Buf()
        f.dma("sp", wtm[:], self.WINTM[l], reads=[self.bW[("wintm", l)]], writes=[bwtm])
        wq32 = f.sb("wq32", [128, 2, 384], F32); wk32 = f.sb("wk32", [128, 512], F32); bw32 = Buf()
        gq = f.sb("gq", [128, 3], F32)
        wq16 = f.sb("wq16", [128, 2, 384], BF16); wk16 = f.sb("wk16", [128, 512], BF16); bw16 = Buf()
        f.dma("sp", wq32[:], self.wuq[l].rearrange("(kc p) c -> p kc c", p=128), writes=[bw32])
        f.dma("sp", wk32[:], self.wukv[l], joins=[bw32])
        f.dma("sp", gq[:, 0:2], self.mqn[l].rearrange("(kc p) -> p kc", p=128), joins=[bw32], allow_slow_non_contiguous=True)
        f.dma("sp", gq[:, 2:3], self.mkn[l].rearrange("(kc p) -> p kc", p=128), joins=[bw32], allow_slow_non_contiguous=True)
        for kc in range(2):
            kw = dict(writes=[bw16]) if kc == 0 else dict(joins=[bw16])
            f.op("dve", lambda: nc.vector.tensor_scalar(out=wq16[:, kc, :], in0=wq32[:, kc, :], scalar1=gq[:, kc:kc + 1], scalar2=None,
                                                        op0=ALU.mult), reads=[bw32], **kw)
        f.op("dve", lambda: nc.vector.tensor_scalar(out=wk16[:], in0=wk32[:], scalar1=gq[:, 2:3], scalar2=None, op0=ALU.mult),
             reads=[bw32], joins=[bw16])
        gqk = f.sb("gqk", [128, 10, 64], F32); bgqk = Buf()
        for i in range(10):
            src = (self.gqn if i < 8 else self.gkn)[l].partition_broadcast(128)
            kw = dict(writes=[bgqk]) if i == 0 else dict(joins=[bgqk])
            f.dma("sp", gqk[:, i, :], src, **kw)
        hT = f.sb("hT", [128, 8, 1024], BF16); bhT = Buf()
        cdT = f.sb("cdT", [128, 3, 1024], BF16); bcdT = Buf()
        wfm = [f.sb("wfm%d" % i, [128, 8, 128], BF16) for i in range(3)]; bwfm = [Buf() for _ in range(3)]
        z = f.sb("z", [128, 1440], F32); bz = Buf()
        rp = f.sb("rp", [128, 96], F32); brp = Buf()
        sq = f.sb("sq", [128, 640], F32); bsq = Buf()
        ss = f.sb("ss", [128, 16], F32); bss = Buf()
        xn = f.sb("xn", [128, 640], F32); bxn = Buf()
        tA = f.sb("tA", [128, 320], F32); tB = f.sb("tB", [128, 320], F32); btAB = Buf()
        qk16 = f.sb("qk16", [128, 768], BF16); bqk16 = Buf()
        qcf = f.sb("qcf", [128, 384], F32); kvf = f.sb("kvf", [128, 512], F32); bqcf = Buf()
        kpe = f.sb("kpe", [128, 32], F32); bkpe = Buf()
        qf16 = f.sb("qf16", [128, 4, 96], BF16); kf16 = f.sb("kf16", [128, 4, 96], BF16); bf16b = Buf()
        junk = f.sb("junk", [128, 256], F32); bjunk = Buf()
        qT_sb = f.sb("qT_sb", [128, 4, 1024], BF16); kT_sb = f.sb("kT_sb", [128, 2, 1024], BF16)
        cqT_sb = f.sb("cqT_sb", [96, 4, 1024], BF16); ckT_sb = f.sb("ckT_sb", [96, 4, 1024], BF16); bT_sb = Buf()
        av1 = f.sb("av1", [128, 8, 4, 65], BF16); bv1 = f.sb("bv1", [128, 8, 2, 65], BF16); cv1 = f.sb("cv1", [128, 8, 4, 65], BF16)
        bv_sb = Buf()
        ev16 = [f.sb("ev16_%d" % i, [128, 512], BF16) for i in range(3)]; bev16 = [Buf() for _ in range(3)]
        f.op("dve", lambda: nc.vector.memset(av1[:], 1.0), writes=[bv_sb])
        f.op("dve", lambda: nc.vector.memset(bv1[:], 1.0), joins=[bv_sb])
        f.op("dve", lambda: nc.vector.memset(cv1[:], 1.0), joins=[bv_sb])
        self.new_gen(self.bA, self.bGT)
        nsb = T // 1024
        evi = 0
        fmi = 0
        for sb in range(nsb):
            self.load_hT(hT, bhT, sb, 8)
            tok0 = sb * 1024

            def fm_chunk(m, consume):
                nonlocal fmi
                w, bw = wfm[fmi % 3], bwfm[fmi % 3]
                fmi += 1
                f.dma("sp", w[:], self.WINFM[l, m], reads=[self.bW[("winfm", l, m)]], writes=[bw])
                for tb in range(2):
                    bank = (fmi * 2 + tb) % 3
                    for kc in range(8):
                        kw = dict(writes=[self.bpb[bank]]) if kc == 0 else dict(joins=[self.bpb[bank]])
                        f.op("pe", lambda: nc.tensor.matmul(self.pb[bank][:], lhsT=w[:, kc, :], rhs=hT[:, kc, tb * 512:(tb + 1) * 512],
                                                            start=(kc == 0), stop=(kc == 7)), reads=[bw, bhT], **kw)
                    consume(tb, bank)

            for i, m in enumerate((4, 5, 6)):
                def cons(tb, bank, i=i):
                    kw = dict(writes=[bcdT]) if (i == 0 and tb == 0) else dict(joins=[bcdT])
                    f.op("act", lambda: nc.scalar.copy(out=cdT[:, i, tb * 512:(tb + 1) * 512], in_=self.pb[bank][:]),
                         reads=[self.bpb[bank]], **kw)
                fm_chunk(m, cons)
            first_sbw = True
            for tt in range(8):
                gt = sb * 8 + tt
                tsl = slice(tt * 128, (tt + 1) * 128)
                for n, (c0, cw) in enumerate(((0, 512), (512, 512), (1024, 416))):
                    bank = 3 + n
                    for kc in range(8):
                        kw = dict(writes=[self.bpb[bank]]) if kc == 0 else dict(joins=[self.bpb[bank]])
                        f.op("pe", lambda: nc.tensor.matmul(self.pb[bank][:, 0:cw], lhsT=hT[:, kc, tsl], rhs=wtm[:, kc, c0:c0 + cw],
                                                            start=(kc == 0), stop=(kc == 7)), reads=[bhT, bwtm], **kw)
                    kw = dict(writes=[bz]) if n == 0 else dict(joins=[bz])
                    if n == 1:
                        f.op("dve", lambda: nc.vector.tensor_copy(out=z[:, c0:c0 + cw], in_=self.pb[bank][:, 0:cw]), reads=[self.bpb[bank]], **kw)
                    else:
                        f.op("act", lambda: nc.scalar.copy(out=z[:, c0:c0 + cw], in_=self.pb[bank][:, 0:cw]), reads=[self.bpb[bank]], **kw)
                f.dma("sp", rp[:], self.rope[gt * 128:(gt + 1) * 128, :], writes=[brp])
                kwv = dict(writes=[bv_sb]) if first_sbw else dict(joins=[bv_sb])
                kwT = dict(writes=[bT_sb]) if first_sbw else dict(joins=[bT_sb])
                first_sbw = False
                f.op("act", lambda: nc.scalar.copy(out=av1[:, tt, :, 0:64], in_=z[:, 0:256].rearrange("p (h d) -> p h d", h=4)), reads=[bz], **kwv)
                f.op("act", lambda: nc.scalar.copy(out=bv1[:, tt, :, 0:64], in_=z[:, 896:1024].rearrange("p (h d) -> p h d", h=2)), reads=[bz], joins=[bv_sb])
                zqk = z[:, 256:896]
                f.op("dve", lambda: nc.vector.tensor_tensor(out=sq[:], in0=zqk, in1=zqk, op=ALU.mult), reads=[bz], writes=[bsq])
                f.op("dve", lambda: nc.vector.tensor_reduce(out=ss[:, 0:10], in_=sq[:].rearrange("p (h d) -> p h d", h=10), axis=AX.X, op=ALU.add),
                     reads=[bsq], writes=[bss])
                f.op("act", lambda: nc.scalar.activation(out=ss[:, 0:10], in_=ss[:, 0:10], func=AF.Sqrt, bias=self.epsc[:, 1:2], scale=1.0 / 64),
                     reads=[bss], writes=[bss])
                f.op("dve", lambda: nc.vector.reciprocal(out=ss[:, 0:10], in_=ss[:, 0:10]), reads=[bss], writes=[bss])
                xn3 = xn[:].rearrange("p (h d) -> p h d", h=10)
                f.op("dve", lambda: nc.vector.tensor_tensor(out=xn3, in0=zqk.rearrange("p (h d) -> p h d", h=10),
                                                            in1=ss[:, 0:10].unsqueeze(2).broadcast_to([128, 10, 64]), op=ALU.mult),
                     reads=[bz, bss], writes=[bxn])
                f.op("dve", lambda: nc.vector.tensor_tensor(out=xn3, in0=xn3, in1=gqk[:], op=ALU.mult), reads=[bxn, bgqk], writes=[bxn])
                x1, x2 = xn3[:, :, 0:32], xn3[:, :, 32:64]
                cb = rp[:, 0:32].unsqueeze(1).broadcast_to([128, 10, 32]); sbn = rp[:, 32:64].unsqueeze(1).broadcast_to([128, 10, 32])
                tA3 = tA[:].rearrange("p (h d) -> p h d", h=10); tB3 = tB[:].rearrange("p (h d) -> p h d", h=10)
                q16 = qk16[:, 0:512].rearrange("p (h d) -> p h d", h=8)
                k16 = qk16[:, 512:768].rearrange("p (k r d) -> p k r d", k=2, r=2)
                f.op("dve", lambda: nc.vector.tensor_tensor(out=tA3, in0=x1, in1=cb, op=ALU.mult), reads=[bxn, brp], writes=[btAB])
                f.op("dve", lambda: nc.vector.tensor_tensor(out=tB3, in0=x2, in1=sbn, op=ALU.mult), reads=[bxn, brp], joins=[btAB])
                f.op("dve", lambda: nc.vector.tensor_tensor(out=q16[:, :, 0:32], in0=tA3[:, 0:8, :], in1=tB3[:, 0:8, :], op=ALU.subtract),
                     reads=[btAB], writes=[bqk16])
                for r in range(2):
                    f.op("dve", lambda: nc.vector.tensor_tensor(out=k16[:, :, r, 0:32], in0=tA3[:, 8:10, :], in1=tB3[:, 8:10, :], op=ALU.subtract),
                         reads=[btAB], joins=[bqk16])
                f.op("dve", lambda: nc.vector.tensor_tensor(out=tA3, in0=x2, in1=cb, op=ALU.mult), reads=[bxn, brp, bqk16], writes=[btAB])
                f.op("dve", lambda: nc.vector.tensor_tensor(out=tB3, in0=x1, in1=sbn, op=ALU.mult), reads=[bxn, brp], joins=[btAB])
                f.op("dve", lambda: nc.vector.tensor_tensor(out=q16[:, :, 32:64], in0=tA3[:, 0:8, :], in1=tB3[:, 0:8, :], op=ALU.add),
                     reads=[btAB], joins=[bqk16])
                for r in range(2):
                    f.op("dve", lambda: nc.vector.tensor_tensor(out=k16[:, :, r, 32:64], in0=tA3[:, 8:10, :], in1=tB3[:, 8:10, :], op=ALU.add),
                         reads=[btAB], joins=[bqk16])
                for j in range(4):
                    kw = dict(writes=[self.bpt[0]]) if j == 0 else dict(joins=[self.bpt[0]])
                    f.op("pe", lambda: nc.tensor.transpose(out=self.pT[0][:, j * 128:(j + 1) * 128], in_=qk16[:, j * 128:(j + 1) * 128], identity=self.ident[:]),
                         reads=[bqk16, self.bid], **kw)
                f.op("act", lambda: nc.scalar.copy(out=qT_sb[:, :, tsl], in_=self.pT[0][:].rearrange("p (c t) -> p c t", c=4)),
                     reads=[self.bpt[0]], **kwT)
                for j in range(2):
                    kw = dict(writes=[self.bpt[1]]) if j == 0 else dict(joins=[self.bpt[1]])
                    f.op("pe", lambda: nc.tensor.transpose(out=self.pT[1][:, j * 128:(j + 1) * 128], in_=qk16[:, 512 + j * 128:512 + (j + 1) * 128],
                                                           identity=self.ident[:]), reads=[bqk16, self.bid], **kw)
                f.op("act", lambda: nc.scalar.copy(out=kT_sb[:, :, tsl], in_=self.pT[1][:, 0:256].rearrange("p (c t) -> p c t", c=2)),
                     reads=[self.bpt[1]], joins=[bT_sb])
                f.op("act", lambda: nc.scalar.activation(out=junk[:, 0:256], in_=z[:, 1024:1280], func=AF.Square, accum_out=ss[:, 10:11]),
                     reads=[bz, bss], writes=[bjunk], joins=[bss])
                f.op("act", lambda: nc.scalar.activation(out=junk[:, 0:128], in_=z[:, 1280:1408], func=AF.Square, accum_out=ss[:, 11:12]),
                     reads=[bz], writes=[bjunk], joins=[bss])
                f.op("act", lambda: nc.scalar.activation(out=ss[:, 10:11], in_=ss[:, 10:11], func=AF.Sqrt, bias=self.epsc[:, 1:2], scale=1.0 / 256),
                     reads=[bss], writes=[bss])
                f.op("act", lambda: nc.scalar.activation(out=ss[:, 11:12], in_=ss[:, 11:12], func=AF.Sqrt, bias=self.epsc[:, 1:2], scale=1.0 / 128),
                     reads=[bss], writes=[bss])
                f.op("dve", lambda: nc.vector.reciprocal(out=ss[:, 10:12], in_=ss[:, 10:12]), reads=[bss], writes=[bss])
                for kc in range(2):
                    kw = dict(writes=[self.bpb[0]]) if kc == 0 else dict(joins=[self.bpb[0]])
                    f.op("pe", lambda: nc.tensor.matmul(self.pb[0][:, 0:384], lhsT=cdT[:, kc, tsl], rhs=wq16[:, kc, :], start=(kc == 0), stop=(kc == 1)),
                         reads=[bcdT, bw16], **kw)
                f.op("pe", lambda: nc.tensor.matmul(self.pb[1][:, 0:512], lhsT=cdT[:, 2, tsl], rhs=wk16[:], start=True, stop=True),
                     reads=[bcdT, bw16], writes=[self.bpb[1]])
                f.op("dve", lambda: nc.vector.tensor_scalar(out=qcf[:], in0=self.pb[0][:, 0:384], scalar1=ss[:, 10:11], scalar2=None, op0=ALU.mult),
                     reads=[self.bpb[0], bss], writes=[bqcf])
                f.op("dve", lambda: nc.vector.tensor_scalar(out=kvf[:], in0=self.pb[1][:, 0:512], scalar1=ss[:, 11:12], scalar2=None, op0=ALU.mult),
                     reads=[self.bpb[1], bss], joins=[bqcf])
                qc3 = qcf[:].rearrange("p (h d) -> p h d", h=4)
                kv3 = kvf[:].rearrange("p (h d) -> p h d", h=4)
                cc = rp[:, 64:80]; sc = rp[:, 80:96]
                cc4 = cc.unsqueeze(1).broadcast_to([128, 4, 16]); sc4 = sc.unsqueeze(1).broadcast_to([128, 4, 16])
                tq = tA[:, 0:64].rearrange("p (h d) -> p h d", h=4); tq2 = tB[:, 0:64].rearrange("p (h d) -> p h d", h=4)
                q1, q2 = qc3[:, :, 64:80], qc3[:, :, 80:96]
                f.op("act", lambda: nc.scalar.copy(out=qf16[:, :, 0:64], in_=qc3[:, :, 0:64]), reads=[bqcf], writes=[bf16b])
                f.op("dve", lambda: nc.vector.tensor_tensor(out=tq, in0=q1, in1=cc4, op=ALU.mult), reads=[bqcf, brp, bqk16], writes=[btAB])
                f.op("dve", lambda: nc.vector.tensor_tensor(out=tq2, in0=q2, in1=sc4, op=ALU.mult), reads=[bqcf, brp], joins=[btAB])
                f.op("dve", lambda: nc.vector.tensor_tensor(out=qf16[:, :, 64:80], in0=tq, in1=tq2, op=ALU.subtract), reads=[btAB], joins=[bf16b])
                f.op("dve", lambda: nc.vector.tensor_tensor(out=tq, in0=q2, in1=cc4, op=ALU.mult), reads=[bqcf, brp, bf16b], writes=[btAB])
                f.op("dve", lambda: nc.vector.tensor_tensor(out=tq2, in0=q1, in1=sc4, op=ALU.mult), reads=[bqcf, brp], joins=[btAB])
                f.op("dve", lambda: nc.vector.tensor_tensor(out=qf16[:, :, 80:96], in0=tq, in1=tq2, op=ALU.add), reads=[btAB], joins=[bf16b])
                k1, k2 = z[:, 1408:1424], z[:, 1424:1440]
                t1, t2 = tA[:, 64:80], tB[:, 64:80]
                f.op("dve", lambda: nc.vector.tensor_tensor(out=t1, in0=k1, in1=cc, op=ALU.mult), reads=[bz, brp, bf16b], writes=[btAB])
                f.op("dve", lambda: nc.vector.tensor_tensor(out=t2, in0=k2, in1=sc, op=ALU.mult), reads=[bz, brp], joins=[btAB])
                f.op("dve", lambda: nc.vector.tensor_tensor(out=kpe[:, 0:16], in0=t1, in1=t2, op=ALU.subtract), reads=[btAB], writes=[bkpe])
                f.op("dve", lambda: nc.vector.tensor_tensor(out=t1, in0=k2, in1=cc, op=ALU.mult), reads=[bz, brp, bkpe], writes=[btAB])
                f.op("dve", lambda: nc.vector.tensor_tensor(out=t2, in0=k1, in1=sc, op=ALU.mult), reads=[bz, brp], joins=[btAB])
                f.op("dve", lambda: nc.vector.tensor_tensor(out=kpe[:, 16:32], in0=t1, in1=t2, op=ALU.add), reads=[btAB], joins=[bkpe])
                f.op("act", lambda: nc.scalar.copy(out=kf16[:, :, 0:64], in_=kv3[:, :, 0:64]), reads=[bqcf], joins=[bf16b])
                f.op("dve", lambda: nc.vector.tensor_copy(out=kf16[:, :, 64:96], in_=kpe[:].unsqueeze(1).broadcast_to([128, 4, 32])),
                     reads=[bkpe], joins=[bf16b])
                f.op("act", lambda: nc.scalar.copy(out=cv1[:, tt, :, 0:64], in_=kv3[:, :, 64:128]), reads=[bqcf], joins=[bv_sb])
                for which, (src16, dstT) in enumerate(((qf16, cqT_sb), (kf16, ckT_sb))):
                    bp = self.bpt[which]
                    for h in range(4):
                        kw = dict(writes=[bp]) if h == 0 else dict(joins=[bp])
                        f.op("pe", lambda: nc.tensor.transpose(out=self.pT[which][0:96, h * 128:(h + 1) * 128],
                                                               in_=src16[:, h, :], identity=self.ident[:]), reads=[bf16b, self.bid], **kw)
                    f.op("dve" if which == 0 else "act",
                         (lambda: nc.vector.tensor_copy(out=dstT[:, :, tsl], in_=self.pT[which][0:96, :].rearrange("p (c t) -> p c t", c=4)))
                         if which == 0 else
                         (lambda: nc.scalar.copy(out=dstT[:, :, tsl], in_=self.pT[which][0:96, :].rearrange("p (c t) -> p c t", c=4))),
                         reads=[bp], joins=[bT_sb])
            tsb = slice(tok0, tok0 + 1024)
            f.dma("sp", self.BQT[:, :, tsb].rearrange("c p t -> p c t"), qT_sb[:], reads=[bT_sb], joins=[self.bA])
            f.dma("sp", self.BKT[:, :, tsb].rearrange("c p t -> p c t"), kT_sb[:], reads=[bT_sb], joins=[self.bA])
            f.dma("sp", self.CQT[:, :, tsb].rearrange("c p t -> p c t"), cqT_sb[:], reads=[bT_sb], joins=[self.bA])
            f.dma("sp", self.CKT[:, :, tsb].rearrange("c p t -> p c t"), ckT_sb[:], reads=[bT_sb], joins=[self.bA])
            f.dma("sp", self.AV1[tsb].rearrange("(n p) h d -> p n h d", p=128), av1[:], reads=[bv_sb], joins=[self.bA])
            f.dma("sp", self.BV1[tsb].rearrange("(n p) h d -> p n h d", p=128), bv1[:], reads=[bv_sb], joins=[self.bA])
            f.dma("sp", self.CV1[tsb].rearrange("(n p) h d -> p n h d", p=128), cv1[:], reads=[bv_sb], joins=[self.bA])
            for m in (0, 1, 2, 3):
                def cons(tb, bank, m=m):
                    nonlocal evi
                    e16, be = ev16[evi % 3], bev16[evi % 3]
                    evi += 1
                    f.op("dve", lambda: nc.vector.tensor_copy(out=e16[:], in_=self.pb[bank][:]), reads=[self.bpb[bank]], writes=[be])
                    dst = (self.AQT if m < 2 else self.AKT)[m % 2, :, tok0 + tb * 512: tok0 + (tb + 1) * 512]
                    f.dma("sp", dst, e16[:], reads=[be], joins=[self.bA])
                fm_chunk(m, cons)
            for g in range(24):
                def cons(tb, bank, g=g):
                    nonlocal evi
                    e16, be = ev16[evi % 3], bev16[evi % 3]
                    evi += 1
                    f.op("act", lambda: nc.scalar.activation(out=e16[:], in_=self.pb[bank][:], func=AF.Sigmoid), reads=[self.bpb[bank]], writes=[be])
                    f.dma("sp", self.GT[g, :, tok0 + tb * 512: tok0 + (tb + 1) * 512], e16[:], reads=[be], joins=[self.bGT])
                fm_chunk(7 + g, cons)
        f.barrier()
        f.pop()

    def full_attn(self, l, QT, KT_of, V1, nheads, kdim, scale, y0, kv_of, qsel):
        f, nc = self.f, self.nc
        T = self.T
        f.push()
        kT = f.sb("kT", [128, S], BF16); bkT = Buf()
        v1 = f.sb("v1", [128, 32, 65], BF16); bv = Buf()
        qT = [f.sb("qT%d" % i, [128, 512], BF16) for i in range(4)]; bq = [Buf() for _ in range(4)]
        for i in range(4):
            f.op("dve", lambda: nc.vector.memset(qT[i][:], 0.0), writes=[bq[i]])
        pT = [f.sb("pT%d" % i, [128, 512], BF16) for i in range(3)]; bp = [Buf() for _ in range(3)]
        rc = f.sb("rc", [128, 512], F32); brc = Buf()
        f.op("dve", lambda: nc.vector.memset(rc[:], 0.0), writes=[brc])
        fullk = (kdim == 64)
        kps_mm = slice(0, 128) if fullk else slice(0, kdim)
        rb = f.sb("rb", [64, 512], F32); brb = Buf()
        yo = [f.sb("yo%d" % i, [64, 512], BF16) for i in range(2)]; byo = [Buf(), Buf()]
        nqb = T // 512
        cur_kv = None
        it = 0
        for h in range(nheads):
            kv = kv_of(h)
            qc, ps = qsel(h)
            if kv != cur_kv:
                cur_kv = kv
                ksrc, kps = KT_of(kv)
                f.dma("sp", kT[kps, :], ksrc, reads=[self.bA], writes=[bkT])
                f.dma("sp", v1[:], V1[:, kv, :].rearrange("(n p) d -> p n d", p=128), reads=[self.bA], writes=[bv])
            for qb in range(nqb):
                qi = (it % 2) + (2 if ps.start else 0)
                q, bqq = qT[qi], bq[qi]
                f.dma("sp", q[ps, :], QT[qc, ps, qb * 512:(qb + 1) * 512], reads=[self.bA], writes=[bqq])
                obank = 4 + (it % 2)
                bo = self.bpb[obank]
                NK = S // 128

                def s_mm(kt):
                    bank = kt % 4
                    f.op("pe", lambda: nc.tensor.matmul(self.pb[bank][:], lhsT=kT[kps_mm, kt * 128:(kt + 1) * 128], rhs=q[kps_mm, :], start=True, stop=True),
                         reads=[bkT, bqq], writes=[self.bpb[bank]])
                    p, bpp = pT[kt % 3], bp[kt % 3]
                    f.op("act", lambda: nc.scalar.activation(out=p[:], in_=self.pb[bank][:], func=AF.Exp, scale=scale),
                         reads=[self.bpb[bank]], writes=[bpp])

                def pv_mm(kt):
                    p, bpp = pT[kt % 3], bp[kt % 3]
                    kw = dict(writes=[bo]) if kt == 0 else dict(joins=[bo])
                    f.op("pe", lambda: nc.tensor.matmul(self.pb[obank][0:65, :], lhsT=v1[:, kt, :], rhs=p[:], start=(kt == 0), stop=(kt == NK - 1)),
                         reads=[bv, bpp], **kw)
                s_mm(0)
                s_mm(1)
                for kt in range(NK):
                    if kt + 2 < NK:
                        s_mm(kt + 2)
                    pv_mm(kt)
                f.op("dve", lambda: nc.vector.reciprocal(out=rc[64:65, :], in_=self.pb[obank][64:65, :]), reads=[bo], writes=[brc])
                f.op("pe", lambda: nc.tensor.matmul(self.pb[3][0:64, :], lhsT=self.sel64[:], rhs=rc[:], start=True, stop=True),
                     reads=[brc, self.bones], writes=[self.bpb[3]])
                f.op("dve", lambda: nc.vector.tensor_copy(out=rb[:], in_=self.pb[3][0:64, :]), reads=[self.bpb[3]], writes=[brb])
                y, by = yo[it % 2], byo[it % 2]
                f.op("dve", lambda: nc.vector.tensor_tensor(out=y[:], in0=self.pb[obank][0:64, :], in1=rb[:], op=ALU.mult), reads=[bo, brb], writes=[by])
                f.dma("sp", self.YT[y0 + h, :, qb * 512:(qb + 1) * 512], y[:], reads=[by], joins=[self.bYT])
                it += 1
        f.barrier()
        f.pop()

    def na_attn(self, l):
        f, nc = self.f, self.nc
        T = self.T
        f.push()
        nbI = f.sb("nbI", [128, 5, 512], F32); bnbI = Buf()
        nbE = f.sb("nbE", [128, 5, 512], F32); bnbE = Buf()
        f.dma("sp", nbI[:], self.nab[l, 2].rearrange("t p c -> p t c"), writes=[bnbI])
        kT = f.sb("nkT", [128, 2, 640], BF16); bkT = [Buf(), Buf()]
        kTb = f.sb("nkTb", [128, 2, 640], BF16)
        v1 = [f.sb("nv1_%d" % i, [128, 5, 4, 65], BF16) for i in range(2)]; bv = [Buf(), Buf()]
        qT = [f.sb("nqT%d" % i, [128, 4, 128], BF16) for i in range(2)]; bq = [Buf(), Buf()]
        for i in range(2):
            f.op("dve", lambda: nc.vector.memset(qT[i][:], 0.0), writes=[bq[i]])
        sc = [f.sb("nsc%d" % i, [128, 512], F32) for i in range(2)]; bsc = [Buf(), Buf()]
        pT = [f.sb("npT%d" % i, [128, 512], BF16) for i in range(2)]; bp = [Buf(), Buf()]
        rc = f.sb("nrc", [128, 512], F32); brc = Buf()
        f.op("dve", lambda: nc.vector.memset(rc[:], 0.0), writes=[brc])
        rb = f.sb("nrb", [64, 512], F32); brb = Buf()
        yo = [f.sb("nyo%d" % i, [64, 4, 128], BF16) for i in range(2)]; byo = [Buf(), Buf()]
        kTs = [kT, kTb]
        nblk = T // 128
        pat_of = {0: 0, 1: 1, 30: 3, 31: 4}
        si = 0
        for j in range(nblk):
            i2 = j % 2
            k0 = min(max(j - 2, 0), 27)
            kt_, bk_ = kTs[i2], bkT[i2]
            f.dma("sp", kt_[:], self.AKT[:, :, k0 * 128:(k0 + 5) * 128].rearrange("c p t -> p c t"), reads=[self.bA], writes=[bk_])
            f.dma("sp", v1[i2][:], self.AV1[k0 * 128:(k0 + 5) * 128].rearrange("(n p) h d -> p n h d", p=128), reads=[self.bA], writes=[bv[i2]])
            for hf in range(2):
                kwq = dict(writes=[bq[i2]]) if hf == 0 else dict(joins=[bq[i2]])
                f.dma("sp", qT[i2][hf * 64:(hf + 1) * 64, :, :].rearrange("p (c two) t -> p c two t", two=2)[:, :, hf, :],
                      self.AQT[:, hf * 64:(hf + 1) * 64, j * 128:(j + 1) * 128].rearrange("c p t -> p c t"), reads=[self.bA], **kwq)
            if j in pat_of:
                f.dma("sp", nbE[:], self.nab[l, pat_of[j]].rearrange("t p c -> p t c"), writes=[bnbE])
                nb, bnb = nbE, bnbE
            else:
                nb, bnb = nbI, bnbI
            for t in range(5):
                sbank = si % 2
                for h in range(4):
                    c = h // 2
                    kw = dict(writes=[self.bpb[sbank]]) if h == 0 else dict(joins=[self.bpb[sbank]])
                    f.op("pe", lambda: nc.tensor.matmul(self.pb[sbank][:, h * 128:(h + 1) * 128], lhsT=kt_[:, c, t * 128:(t + 1) * 128],
                                                        rhs=qT[i2][:, h, :], start=True, stop=True), reads=[bk_, bq[i2]], **kw)
                s_, bs_ = sc[si % 2], bsc[si % 2]
                p_, bp_ = pT[si % 2], bp[si % 2]
                f.op("dve", lambda: nc.vector.scalar_tensor_tensor(out=s_[:], in0=self.pb[sbank][:], scalar=0.125, in1=nb[:, t, :],
                                                                    op0=ALU.mult, op1=ALU.add), reads=[self.bpb[sbank], bnb], writes=[bs_])
                f.op("act", lambda: nc.scalar.activation(out=p_[:], in_=s_[:], func=AF.Exp), reads=[bs_], writes=[bp_])
                for h in range(4):
                    bo = self.bpb[2 + h]
                    kw = dict(writes=[bo]) if t == 0 else dict(joins=[bo])
                    f.op("pe", lambda: nc.tensor.matmul(self.pb[2 + h][0:65, 0:128], lhsT=v1[i2][:, t, h, :], rhs=p_[:, h * 128:(h + 1) * 128],
                                                        start=(t == 0), stop=(t == 4)), reads=[bv[i2], bp_], **kw)
                si += 1
            for h in range(4):
                kw = dict(writes=[brc]) if h == 0 else dict(joins=[brc])
                f.op("dve", lambda: nc.vector.reciprocal(out=rc[64:65, h * 128:(h + 1) * 128], in_=self.pb[2 + h][64:65, 0:128]),
                     reads=[self.bpb[2 + h]], **kw)
            f.op("pe", lambda: nc.tensor.matmul(self.pb[0][0:64, :], lhsT=self.sel64[:], rhs=rc[:], start=True, stop=True),
                 reads=[brc, self.bones], writes=[self.bpb[0]])
            f.op("act", lambda: nc.scalar.copy(out=rb[:], in_=self.pb[0][0:64, :]), reads=[self.bpb[0]], writes=[brb])
            y, by = yo[i2], byo[i2]
            for h in range(4):
                kw = dict(writes=[by]) if h == 0 else dict(joins=[by])
                f.op("dve", lambda: nc.vector.tensor_tensor(out=y[:, h, :], in0=self.pb[2 + h][0:64, 0:128], in1=rb[:, h * 128:(h + 1) * 128], op=ALU.mult),
                     reads=[self.bpb[2 + h], brb], **kw)
            f.dma("sp", self.YT[0:4, :, j * 128:(j + 1) * 128].rearrange("h p t -> p h t"), y[:], reads=[by], joins=[self.bYT])
        f.barrier()
        f.pop()

    def phase_B(self, l):
        import os
        parts = os.environ.get("BPARTS", "ngm")
        self.new_gen(self.bYT)
        if "n" in parts:
            self.na_attn(l)
        if "g" in parts:
            self.full_attn(l, self.BQT, lambda kv: (self.BKT[kv], slice(0, 128)), self.BV1, 8, 64, 0.125, 4,
                           lambda h: h // 4, lambda h: (h // 2, slice((h % 2) * 64, (h % 2) * 64 + 64)))
        if "m" in parts:
            self.full_attn(l, self.CQT, lambda kv: (self.CKT[kv], slice(0, 96)), self.CV1, 4, 96, float(96 ** -0.5), 12,
                           lambda h: h, lambda h: (h, slice(0, 96)))

    def out_proj_ln(self, srcT, bsrc, w, bw, gt, tt, nk):
        f, nc = self.f, self.nc
        banks = (4, 5)
        for hf in range(2):
            bank = banks[hf]
            for kc in range(nk):
                kw = dict(writes=[self.bpb[bank]]) if kc == 0 else dict(joins=[self.bpb[bank]])
                f.op("pe", lambda: nc.tensor.matmul(self.pb[bank][:], lhsT=srcT[:, kc, tt * 128:(tt + 1) * 128], rhs=w[:, kc, hf * 512:(hf + 1) * 512],
                                                    start=(kc == 0), stop=(kc == nk - 1)), reads=[bsrc, bw], **kw)
        self.ln_tile(gt, [self.pb[4][:], self.pb[5][:]], [self.bpb[4], self.bpb[5]])

    def phase_C(self, l):
        f, nc = self.f, self.nc
        T = self.T
        f.push()
        self.load_ln_params(self.ln_g[l, 0], self.ln_b[l, 0])
        wp = f.sb("wp", [128, 8, D], BF16); bwp = Buf()
        wo = f.sb("wo", [128, 8, D], BF16); bwo = Buf()
        f.dma("sp", wp[:, 0:2, :], self.WPA[l], reads=[self.bW[("wpa", l)]], writes=[bwp])
        f.dma("sp", wp[:, 2:6, :], self.WPB[l], reads=[self.bW[("wpb", l)]], joins=[bwp])
        f.dma("sp", wp[:, 6:8, :], self.WPC[l], reads=[self.bW[("wpc", l)]], joins=[bwp])
        f.dma("sp", wo[:], self.WOUT[l], reads=[self.bW[("wout", l)]], writes=[bwo])
        yT = [f.sb("yT%d" % i, [128, 8, 512], BF16) for i in range(2)]; byT = [Buf(), Buf()]
        gt_ = [f.sb("gts%d" % i, [128, 3, 512], BF16) for i in range(2)]; bgt = [Buf(), Buf()]
        mg = f.sb("mg", [128, 8, 512], BF16); bmg = Buf()
        t1 = f.sb("t1", [128, 512], F32); t2 = f.sb("t2", [128, 512], F32); bt = Buf()
        gi = 0
        for tb in range(T // 512):
            ts = slice(tb * 512, (tb + 1) * 512)
            y, by = yT[tb % 2], byT[tb % 2]
            for c in range(8):
                for two in range(2):
                    kw = dict(writes=[by]) if (c == 0 and two == 0) else dict(joins=[by])
                    f.dma("sp", y[two * 64:(two + 1) * 64, c, :], self.YT[2 * c + two, :, ts], reads=[self.bYT], **kw)
            for m in range(8):
                g, bg = gt_[gi % 2], bgt[gi % 2]
                gi += 1
                f.dma("sp", g[:], self.GT[:, :, ts].rearrange("(b m) p t -> m p b t", m=8)[m], reads=[self.bGT], writes=[bg])
                ms = slice(m * 128, (m + 1) * 128)
                for br, (k0, nk) in enumerate(((0, 2), (2, 4), (6, 2))):
                    bank = br
                    for kc in range(nk):
                        kw = dict(writes=[self.bpb[bank]]) if kc == 0 else dict(joins=[self.bpb[bank]])
                        f.op("pe", lambda: nc.tensor.matmul(self.pb[bank][:], lhsT=wp[:, k0 + kc, ms], rhs=y[:, k0 + kc, :],
                                                            start=(kc == 0), stop=(kc == nk - 1)), reads=[bwp, by], **kw)
                f.op("dve", lambda: nc.vector.tensor_tensor(out=t1[:], in0=self.pb[0][:], in1=g[:, 0, :], op=ALU.mult), reads=[self.bpb[0], bg], writes=[bt])
                f.op("dve", lambda: nc.vector.tensor_tensor(out=t2[:], in0=self.pb[1][:], in1=g[:, 1, :], op=ALU.mult), reads=[self.bpb[1], bg], joins=[bt])
                f.op("dve", lambda: nc.vector.tensor_tensor(out=t1[:], in0=t1[:], in1=t2[:], op=ALU.add), reads=[bt], writes=[bt])
                f.op("dve", lambda: nc.vector.tensor_tensor(out=t2[:], in0=self.pb[2][:], in1=g[:, 2, :], op=ALU.mult), reads=[self.bpb[2], bg, bt], writes=[bt])
                kw = dict(writes=[bmg]) if m == 0 else dict(joins=[bmg])
                f.op("dve", lambda: nc.vector.tensor_tensor(out=mg[:, m, :], in0=t1[:], in1=t2[:], op=ALU.add), reads=[bt], **kw)
            for tt in range(4):
                self.out_proj_ln(mg, bmg, wo, bwo, tb * 4 + tt, tt, 8)
        f.barrier()
        f.pop()

    def phase_D(self, l):
        f, nc = self.f, self.nc
        T = self.T
        f.push()
        self.load_ln_params(self.ln_g[l, 1], self.ln_b[l, 1])
        kmT = f.sb("kmT", [128, 8, 256], BF16); bkm = Buf()
        vm = f.sb("vm", [128, 2, D], BF16); bvm = Buf()
        f.push()
        wkv = f.sb("wkv", [128, 8, 2 * D], BF16); bwkv = Buf()
        memT = f.sb("memT", [128, 8, 256], BF16); bmemT = Buf()
        f.dma("sp", wkv[:], self.WKV[l], reads=[self.bW[("wkv", l)]], writes=[bwkv])
        f.dma("sp", memT[:], self.MEMT, reads=[self.bMEMT], writes=[bmemT])
        for m in range(8):
            bank = m % 4
            for kc in range(8):
                kw = dict(writes=[self.bpb[bank]]) if kc == 0 else dict(joins=[self.bpb[bank]])
                f.op("pe", lambda: nc.tensor.matmul(self.pb[bank][:, 0:256], lhsT=wkv[:, kc, m * 128:(m + 1) * 128], rhs=memT[:, kc, :],
                                                    start=(kc == 0), stop=(kc == 7)), reads=[bwkv, bmemT], **kw)
            kw = dict(writes=[bkm]) if m == 0 else dict(joins=[bkm])
            f.op("dve", lambda: nc.vector.tensor_copy(out=kmT[:, m, :], in_=self.pb[bank][:, 0:256]), reads=[self.bpb[bank]], **kw)
        for mt in range(2):
            for hf in range(2):
                bank = 4 + hf
                for kc in range(8):
                    kw = dict(writes=[self.bpb[bank]]) if kc == 0 else dict(joins=[self.bpb[bank]])
                    f.op("pe", lambda: nc.tensor.matmul(self.pb[bank][:], lhsT=memT[:, kc, mt * 128:(mt + 1) * 128],
                                                        rhs=wkv[:, kc, D + hf * 512: D + (hf + 1) * 512], start=(kc == 0), stop=(kc == 7)),
                         reads=[bwkv, bmemT], **kw)
                kw = dict(writes=[bvm]) if (mt == 0 and hf == 0) else dict(joins=[bvm])
                f.op("act", lambda: nc.scalar.copy(out=vm[:, mt, hf * 512:(hf + 1) * 512], in_=self.pb[bank][:]), reads=[self.bpb[bank]], **kw)
        f.barrier()
        f.pop()
        wq = f.sb("wq", [128, 8, D], BF16); bwq = Buf()
        wo = f.sb("wo", [128, 8, D], BF16); bwo = Buf()
        f.dma("sp", wq[:], self.WQ[l], reads=[self.bW[("wq", l)]], writes=[bwq])
        f.dma("sp", wo[:], self.WO[l], reads=[self.bW[("wo", l)]], writes=[bwo])
        hT = [f.sb("hT%d" % i, [128, 8, 512], BF16) for i in range(2)]; bhT = [Buf(), Buf()]
        qT = f.sb("qT", [128, 8, 512], BF16); bqT = Buf()
        oT = f.sb("oT", [128, 8, 512], BF16); boT = Buf()
        pT = [f.sb("pT%d" % i, [128, 2, 512], BF16) for i in range(2)]; bpT = [Buf(), Buf()]
        rb = f.sb("rb", [128, 512], F32); brb = Buf()
        ntb = T // 512
        self.load_hT(hT[0], bhT[0], 0, 4)
        for tb in range(ntb):
            h_, bh_ = hT[tb % 2], bhT[tb % 2]
            if tb + 1 < ntb:
                self.load_hT(hT[(tb + 1) % 2], bhT[(tb + 1) % 2], tb + 1, 4)
            for m in range(8):
                bank = m % 4
                for kc in range(8):
                    kw = dict(writes=[self.bpb[bank]]) if kc == 0 else dict(joins=[self.bpb[bank]])
                    f.op("pe", lambda: nc.tensor.matmul(self.pb[bank][:], lhsT=wq[:, kc, m * 128:(m + 1) * 128], rhs=h_[:, kc, :],
                                                        start=(kc == 0), stop=(kc == 7)), reads=[bwq, bh_], **kw)
                kw = dict(writes=[bqT]) if m == 0 else dict(joins=[bqT])
                if m % 2 == 0:
                    f.op("dve", lambda: nc.vector.tensor_copy(out=qT[:, m, :], in_=self.pb[bank][:]), reads=[self.bpb[bank]], **kw)
                else:
                    f.op("act", lambda: nc.scalar.copy(out=qT[:, m, :], in_=self.pb[bank][:]), reads=[self.bpb[bank]], **kw)
            for h in range(4):
                p_, bp_ = pT[h % 2], bpT[h % 2]
                for mt in range(2):
                    bank = mt
                    for dc in range(2):
                        kw = dict(writes=[self.bpb[bank]]) if dc == 0 else dict(joins=[self.bpb[bank]])
                        f.op("pe", lambda: nc.tensor.matmul(self.pb[bank][:], lhsT=kmT[:, 2 * h + dc, mt * 128:(mt + 1) * 128], rhs=qT[:, 2 * h + dc, :],
                                                            start=(dc == 0), stop=(dc == 1)), reads=[bkm, bqT], **kw)
                    kw = dict(writes=[bp_]) if mt == 0 else dict(joins=[bp_])
                    f.op("act", lambda: nc.scalar.activation(out=p_[:, mt, :], in_=self.pb[bank][:], func=AF.Exp, scale=1.0 / 16),
                         reads=[self.bpb[bank]], **kw)
                for mt in range(2):
                    kw = dict(writes=[self.bpb[4]]) if mt == 0 else dict(joins=[self.bpb[4]])
                    f.op("pe", lambda: nc.tensor.matmul(self.pb[4][:], lhsT=self.ones16[:], rhs=p_[:, mt, :], start=(mt == 0), stop=(mt == 1)),
                         reads=[self.bones, bp_], **kw)
                f.op("dve", lambda: nc.vector.reciprocal(out=rb[:], in_=self.pb[4][:]), reads=[self.bpb[4]], writes=[brb])
                for dc in range(2):
                    bank = 2 + dc
                    for mt in range(2):
                        kw = dict(writes=[self.bpb[bank]]) if mt == 0 else dict(joins=[self.bpb[bank]])
                        f.op("pe", lambda: nc.tensor.matmul(self.pb[bank][:], lhsT=vm[:, mt, h * 256 + dc * 128: h * 256 + (dc + 1) * 128], rhs=p_[:, mt, :],
                                                            start=(mt == 0), stop=(mt == 1)), reads=[bvm, bp_], **kw)
                    kw = dict(writes=[boT]) if (h == 0 and dc == 0) else dict(joins=[boT])
                    f.op("dve", lambda: nc.vector.tensor_tensor(out=oT[:, 2 * h + dc, :], in0=self.pb[bank][:], in1=rb[:], op=ALU.mult),
                         reads=[self.bpb[bank], brb], **kw)
            for tt in range(4):
                self.out_proj_ln(oT, boT, wo, bwo, tb * 4 + tt, tt, 8)
        f.barrier()
        f.pop()

    def phase_router(self, l):
        f, nc = self.f, self.nc
        f.push()
        wr = f.sb("wr", [128, NE, D], F32); bwr = Buf()
        f.dma("sp", wr[:].rearrange("p e d -> p (e d)"), self.mrt[l // 2].rearrange("e d -> (e d)").partition_broadcast(128), writes=[bwr])
        hb = [f.sb("hb%d" % i, [128, D], F32) for i in range(2)]; bhb = [Buf(), Buf()]
        pr = [f.sb("pr%d" % i, [128, D], F32) for i in range(2)]; bpr = [Buf(), Buf()]
        junk = f.sb("junk", [128, D], F32); bjunk = Buf()
        lg = [f.sb("lg%d" % i, [128, 32], F32) for i in range(2)]; blg = [Buf(), Buf()]
        self.new_gen(self.bGATE)
        pi = 0
        for gt in range(self.NT):
            h, bh = hb[gt % 2], bhb[gt % 2]
            g, bg = lg[gt % 2], blg[gt % 2]
            f.dma("sp", h[:], self.H[gt * 128:(gt + 1) * 128, :], reads=[self.bHt[gt]], writes=[bh])
            for e in range(NE):
                p, bp = pr[pi % 2], bpr[pi % 2]
                pi += 1
                f.op("dve", lambda: nc.vector.tensor_tensor(out=p[:], in0=h[:], in1=wr[:, e, :], op=ALU.mult), reads=[bh, bwr], writes=[bp])
                kw = dict(writes=[bjunk, bg]) if e == 0 else dict(writes=[bjunk], joins=[bg])
                f.op("act", lambda: nc.scalar.activation(out=junk[:], in_=p[:], func=AF.Identity, accum_out=g[:, e:e + 1]),
                     reads=[bp], **kw)
            L = g[:, 0:8]; m1 = g[:, 8:9]; m2 = g[:, 9:10]; k1 = g[:, 10:18]; L2 = g[:, 18:26]; w1 = g[:, 26:27]; w2 = g[:, 27:28]; d = g[:, 28:29]
            k2 = L2
            f.op("dve", lambda: nc.vector.tensor_reduce(out=m1, in_=L, axis=AX.X, op=ALU.max), reads=[bg], writes=[bg])
            f.op("dve", lambda: nc.vector.tensor_scalar(out=k1, in0=L, scalar1=m1, scalar2=None, op0=ALU.is_equal), reads=[bg], writes=[bg])
            f.op("dve", lambda: nc.vector.scalar_tensor_tensor(out=L2, in0=k1, scalar=-1e30, in1=L, op0=ALU.mult, op1=ALU.add), reads=[bg], writes=[bg])
            f.op("dve", lambda: nc.vector.tensor_reduce(out=m2, in_=L2, axis=AX.X, op=ALU.max), reads=[bg], writes=[bg])
            f.op("dve", lambda: nc.vector.tensor_scalar(out=k2, in0=L2, scalar1=m2, scalar2=None, op0=ALU.is_equal), reads=[bg], writes=[bg])
            f.op("dve", lambda: nc.vector.tensor_tensor(out=d, in0=m1, in1=m2, op=ALU.subtract), reads=[bg], writes=[bg])
            f.op("act", lambda: nc.scalar.activation(out=w1, in_=d, func=AF.Sigmoid), reads=[bg], writes=[bg])
            f.op("dve", lambda: nc.vector.tensor_scalar(out=w2, in0=w1, scalar1=-1.0, scalar2=1.0, op0=ALU.mult, op1=ALU.add), reads=[bg], writes=[bg])
            f.op("dve", lambda: nc.vector.tensor_scalar(out=k1, in0=k1, scalar1=w1, scalar2=None, op0=ALU.mult), reads=[bg], writes=[bg])
            f.op("dve", lambda: nc.vector.scalar_tensor_tensor(out=L, in0=k2, scalar=w2, in1=k1, op0=ALU.mult, op1=ALU.add), reads=[bg], writes=[bg])
            f.dma("sp", self.GATE[gt * 128:(gt + 1) * 128, :], L, reads=[bg], joins=[self.bGATE])
        f.barrier()
        f.pop()

    def phase_E(self, l, final):
        f, nc = self.f, self.nc
        T = self.T
        moe = (l % 2 == 1)
        ne = NE if moe else 1
        if moe:
            self.phase_router(l)
        f.push()
        self.load_ln_params(self.ln_g[l, 2], self.ln_b[l, 2])
        hT = f.sb("hT", [128, 8, 1024], BF16); bhT = Buf()
        actT = f.sb("actT", [128, 22, 1024], BF16); bact = Buf()
        wd = f.sb("wd", [128, 22, D], BF16); bwd = Buf()
        wgu = [f.sb("wgu%d" % i, [128, 8, 256], BF16) for i in range(3)]; bwgu = [Buf() for _ in range(3)]
        facc = f.sb("facc", [128, 8, D], F32); bfacc = [Buf() for _ in range(8)]
        gts = f.sb("gts", [128, 8, NE], F32); bgts = Buf()
        sl = [f.sb("sl%d" % i, [128, 512], F32) for i in range(2)]; bsl = [Buf(), Buf()]
        wi = 0
        si = 0
        for sb in range(T // 1024):
            self.load_hT(hT, bhT, sb, 8)
            if moe:
                f.dma("sp", gts[:], self.GATE[sb * 1024:(sb + 1) * 1024, :].rearrange("(n p) e -> p n e", p=128), reads=[self.bGATE], writes=[bgts])
            for e in range(ne):
                ei = self.ebase[l] + e
                f.dma("sp", wd[:], self.WD[ei], reads=[self.bW[("wd", ei)]], writes=[bwd])
                for c in range(22):
                    w, bw = wgu[wi % 3], bwgu[wi % 3]
                    wi += 1
                    f.dma("sp", w[:], self.WGU[ei, c], reads=[self.bW[("wgu", ei, c)]], writes=[bw])
                    for tb in range(2):
                        ts = slice(tb * 512, (tb + 1) * 512)
                        ba, bg_ = (0, 1) if (si % 2 == 0) else (2, 3)
                        for (bank, c0) in ((ba, 0), (bg_, 128)):
                            for kc in range(8):
                                kw = dict(writes=[self.bpb[bank]]) if kc == 0 else dict(joins=[self.bpb[bank]])
                                f.op("pe", lambda: nc.tensor.matmul(self.pb[bank][:], lhsT=w[:, kc, c0:c0 + 128], rhs=hT[:, kc, ts],
                                                                    start=(kc == 0), stop=(kc == 7)), reads=[bw, bhT], **kw)
                        s_, bs_ = sl[si % 2], bsl[si % 2]
                        si += 1
                        f.op("act", lambda: nc.scalar.activation(out=s_[:], in_=self.pb[ba][:], func=AF.Silu), reads=[self.bpb[ba]], writes=[bs_])
                        kw = dict(writes=[bact]) if (c == 0 and tb == 0) else dict(joins=[bact])
                        f.op("dve", lambda: nc.vector.tensor_tensor(out=actT[:, c, ts], in0=s_[:], in1=self.pb[bg_][:], op=ALU.mult),
                             reads=[bs_, self.bpb[bg_]], **kw)
                for tt in range(8):
                    for hf in range(2):
                        bank = 4 + (tt * 2 + hf) % 2
                        for c in range(22):
                            kw = dict(writes=[self.bpb[bank]]) if c == 0 else dict(joins=[self.bpb[bank]])
                            f.op("pe", lambda: nc.tensor.matmul(self.pb[bank][:], lhsT=actT[:, c, tt * 128:(tt + 1) * 128], rhs=wd[:, c, hf * 512:(hf + 1) * 512],
                                                                start=(c == 0), stop=(c == 21)), reads=[bact, bwd], **kw)
                        fa = facc[:, tt, hf * 512:(hf + 1) * 512]
                        bfa = bfacc[tt]
                        if not moe:
                            kw = dict(writes=[bfa]) if hf == 0 else dict(joins=[bfa])
                            f.op("act", lambda: nc.scalar.copy(out=fa, in_=self.pb[bank][:]), reads=[self.bpb[bank]], **kw)
                        elif e == 0:
                            kw = dict(writes=[bfa]) if hf == 0 else dict(joins=[bfa])
                            f.op("dve", lambda: nc.vector.tensor_scalar(out=fa, in0=self.pb[bank][:], scalar1=gts[:, tt, e:e + 1], scalar2=None, op0=ALU.mult),
                                 reads=[self.bpb[bank], bgts], **kw)
                        else:
                            f.op("dve", lambda: nc.vector.scalar_tensor_tensor(out=fa, in0=self.pb[bank][:], scalar=gts[:, tt, e:e + 1], in1=fa,
                                                                                op0=ALU.mult, op1=ALU.add), reads=[self.bpb[bank], bgts, bfa], writes=[bfa])
            for tt in range(8):
                self.ln_tile(sb * 8 + tt, [facc[:, tt, 0:512], facc[:, tt, 512:1024]], [bfacc[tt], bfacc[tt]], dst=self.out if final else None)
        f.barrier()
        f.pop()

    def build(self, stop_after=None, cv_mode=3):
        f = self.f
        self.setup_common()
        for l in range(self.nl):
            if cv_mode & 1:
                self.convert_layer_attn(l)
            if cv_mode & 2:
                self.convert_layer_ffn(l)
        self.bout = Buf("out")

        def dump():
            for gt in range(self.NT):
                rows = slice(gt * 128, (gt + 1) * 128)
                f.dma("sp", self.out[rows, :], self.H[rows, :], reads=[self.bHt[gt]], joins=[self.bout])
        done = False
        self.phase_embed()
        if stop_after == "embed":
            dump(); done = True
        for l in range(self.nl):
            if done:
                break
            for ph, fn in (("A", self.phase_A), ("B", self.phase_B), ("C", self.phase_C), ("D", self.phase_D)):
                fn(l)
                if stop_after == ph and l == self.nl - 1:
                    dump(); done = True
                    break
            if done:
                break
            self.phase_E(l, final=(l == self.nl - 1))
        deps = {}
        _merge(deps, self.bout.w)
        f._wait("sp", deps)
        f.barrier()
        f.close()
        return self.nc


def _rope_table():
    t = np.arange(S)
    row = (t // 64).astype(np.float32)
    col = (t % 64).astype(np.float32)

    def ang(rot):
        n = rot // 4
        inv = (np.float32(10000.0) ** (-np.arange(n, dtype=np.float32) / np.float32(n))).astype(np.float32)
        return np.concatenate([row[:, None] * inv, col[:, None] * inv], axis=-1).astype(np.float32)
    ab, ac = ang(64), ang(32)
    return np.concatenate([np.cos(ab), np.sin(ab), np.cos(ac), np.sin(ac)], axis=-1).astype(np.float32)


def _na_bias_tables(rpb):
    L = rpb.shape[0]
    outt = np.full((L, 5, 5, 128, 4, 128), NEG, np.float32)
    p = np.arange(128)
    for pi, j in enumerate((0, 1, 2, 30, 31)):
        k0 = min(max(j - 2, 0), 27)
        qr = 2 * j + p // 64
        qc = p % 64
        wr = np.clip(qr - 4, 0, 56)
        wc = np.clip(qc - 8, 0, 48)
        for t in range(5):
            kr = 2 * (k0 + t) + p // 64
            kc = p % 64
            valid = ((kr[:, None] >= wr[None, :]) & (kr[:, None] < wr[None, :] + 8) &
                     (kc[:, None] >= wc[None, :]) & (kc[:, None] < wc[None, :] + 16))
            dr = np.clip(kr[:, None] - qr[None, :] + 7, 0, 14)
            dc = np.clip(kc[:, None] - qc[None, :] + 15, 0, 30)
            g = rpb[:, :, dr, dc]
            g = np.where(valid[None, None], g, np.float32(NEG))
            outt[:, pi, t] = g.transpose(0, 2, 1, 3)
    return outt.reshape(L, 5, 5, 128, 512)


_CACHE = {}


def kernel(**inputs):
    inp = {k: np.ascontiguousarray(np.asarray(v)) for k, v in inputs.items()}
    if "prog" not in _CACHE:
        _CACHE["prog"] = Prog().build()
    nc = _CACHE["prog"]
    rope = _rope_table()
    nab = _na_bias_tables(inp["na_rpb"])
    shared = {k: v for k, v in inp.items() if k not in ("x", "mem", "na_rpb", "emb_ln_g", "emb_ln_b")}
    shared["emb_ln_g"] = inp["emb_ln_g"].reshape(1, D)
    shared["emb_ln_b"] = inp["emb_ln_b"].reshape(1, D)
    shared["rope"] = rope
    shared["moe_router_t"] = np.ascontiguousarray(inp["moe_router"].transpose(0, 2, 1))
    del shared["moe_router"]
    shared["nab"] = nab
    in_maps = []
    for c in range(8):
        b = c % 4
        m = dict(shared)
        m["x"] = inp["x"][b]
        m["mem"] = inp["mem"][b]
        in_maps.append(m)
    res = run_bass_kernel_spmd(nc, in_maps, core_ids=list(range(8)))
    return np.stack([res.results[b]["out"] for b in range(4)], axis=0).astype(np.float32)
```

```python
import numpy as np
import concourse.bass as bass
import concourse.mybir as mybir
from concourse.bass_utils import run_bass_kernel_spmd

F32 = mybir.dt.float32
BF16 = mybir.dt.bfloat16
AF = mybir.ActivationFunctionType
ALU = mybir.AluOpType
AX = mybir.AxisListType

D = 1024
S = 4096
DEPTH = 4
INC = 5024
DFF = 2816
NE = 8
ALPHA = float((2 * DEPTH) ** 0.25)
LN_EPS = 1e-5
RMS_EPS = 1e-6
NEG = -30000.0


class Buf:
    __slots__ = ("name", "w", "r", "prev", "x")

    def __init__(self, name="", x=False):
        self.name = name
        self.x = x
        self.w = {}
        self.r = {}
        self.prev = {}


def _merge(d, s):
    for k, v in s.items():
        if d.get(k, 0) < v:
            d[k] = v


class FW:
    def __init__(self, nc, n_main=40, n_cv=16):
        self.nc = nc
        self._cms = []
        self._scopes = []
        self.eng = {"pe": nc.tensor, "act": nc.scalar, "dve": nc.vector, "pool": nc.gpsimd, "sp": nc.sync}
        self.sems = {}
        self.count = {}
        self.known = {e: {} for e in self.eng}
        for e in ("pe", "act", "dve", "pool"):
            self.sems[e] = self._sem("tl_" + e)
            self.count[e] = 0
        self.pools = {"main": [], "cv": []}
        self.rr = {"main": 0, "cv": 0}
        for pn, n in (("main", n_main), ("cv", n_cv)):
            for i in range(n):
                k = "%s%d" % (pn, i)
                self.sems[k] = self._sem(k)
                self.count[k] = 0
                self.pools[pn].append(k)
        self.n_inst = 0
        self.n_wait = 0

    def _sem(self, name):
        cm = self.nc.semaphore(name)
        s = cm.__enter__()
        self._cms.append(cm)
        return s

    def push(self):
        self._scopes.append([])

    def pop(self):
        for cm in reversed(self._scopes.pop()):
            cm.__exit__(None, None, None)

    def _alloc(self, cm):
        t = cm.__enter__()
        (self._scopes[-1] if self._scopes else self._cms).append(cm)
        return t

    def sb(self, name, shape, dt):
        self._uid = getattr(self, "_uid", 0) + 1
        return self._alloc(self.nc.sbuf_tensor("%s_u%d" % (name, self._uid), shape, dt))

    def ps(self, name, shape, dt):
        return self._alloc(self.nc.psum_tensor(name, shape, dt))

    def close(self):
        for cm in reversed(self._cms):
            cm.__exit__(None, None, None)

    def _wait(self, e, deps):
        kn = self.known[e]
        eng = self.eng[e]
        for k, v in deps.items():
            if kn.get(k, 0) >= v:
                continue
            eng.wait_ge(self.sems[k], v)
            kn[k] = v
            self.n_wait += 1

    def _deps(self, reads, writes, joins):
        deps = {}
        for b in reads:
            _merge(deps, b.w)
            if b.x:
                _merge(deps, b.r)
        for b in writes:
            _merge(deps, b.w)
            _merge(deps, b.r)
        for b in joins:
            _merge(deps, b.prev)
            _merge(deps, b.r)
        return deps

    def _commit(self, ev, reads, writes, joins):
        k, v = ev
        for b in writes:
            p = {}
            _merge(p, b.w)
            _merge(p, b.r)
            b.prev = p
            b.w = {k: v}
            b.r = {}
        for b in joins:
            if b.w.get(k, 0) < v:
                b.w[k] = v
        for b in reads:
            if b.r.get(k, 0) < v:
                b.r[k] = v

    def op(self, e, fn, reads=(), writes=(), joins=()):
        deps = self._deps(reads, writes, joins)
        if e == "pe":
            deps.pop("pe", None)
        self._wait(e, deps)
        ins = fn()
        self.count[e] += 1
        ins.then_inc(self.sems[e], 1)
        self._commit((e, self.count[e]), reads, writes, joins)
        self.n_inst += 1
        return ins

    def dma(self, q, out, in_, reads=(), writes=(), joins=(), pool="main", **kw):
        pl = self.pools[pool]
        k = pl[self.rr[pool]]
        self.rr[pool] = (self.rr[pool] + 1) % len(pl)
        deps = self._deps(reads, writes, joins)
        if self.count[k] > 0:
            _merge(deps, {k: self.count[k]})
        self._wait(q, deps)
        ins = self.eng[q].dma_start(out=out, in_=in_, **kw)
        self.count[k] += 16
        ins.then_inc(self.sems[k], 16)
        self._commit((k, self.count[k]), reads, writes, joins)
        self.n_inst += 1
        return ins

    def barrier(self):
        deps = {}
        for e in ("pe", "act", "dve"):
            if self.count[e]:
                deps[e] = self.count[e]
        for k in self.pools["main"]:
            if self.count[k]:
                deps[k] = self.count[k]
        for e in ("pe", "act", "dve", "sp"):
            d = dict(deps)
            d.pop(e, None)
            self._wait(e, d)


class Prog:
    def __init__(self, nlayers=DEPTH, T=S, debug=False, mini=False):
        self.nl = nlayers
        self.T = T
        self.NT = T // 128
        self.debug = debug
        nc = self.nc = bass.Bass("TRN2", target_bir_lowering=False)
        self.f = FW(nc)
        self.used_inputs = []

        def ei(n, s):
            big = n not in ("x", "mem", "emb_ln_g", "emb_ln_b")
            skip = (mini and big) or (nlayers < 2 and n.startswith("moe_"))
            self.used_inputs.append(n) if not skip else None
            if skip:
                return None
            return nc.dram_tensor(n, s, F32, kind="ExternalInput").ap()
        self.x = ei("x", [T, D])
        self.mem = ei("mem", [256, D])
        self.emb_g = ei("emb_ln_g", [1, D])
        self.emb_b = ei("emb_ln_b", [1, D])
        self.w_in = ei("w_in", [DEPTH, D, INC])
        self.nab = ei("nab", [DEPTH, 5, 5, 128, 512])
        self.gqn = ei("gqa_q_norm", [DEPTH, 64])
        self.gkn = ei("gqa_k_norm", [DEPTH, 64])
        self.mqn = ei("mla_q_norm", [DEPTH, 256])
        self.mkn = ei("mla_kv_norm", [DEPTH, 128])
        self.wuq = ei("mla_w_uq", [DEPTH, 256, 384])
        self.wukv = ei("mla_w_ukv", [DEPTH, 128, 512])
        self.wpa = ei("w_proj_a", [DEPTH, 256, D])
        self.wpb = ei("w_proj_b", [DEPTH, 512, D])
        self.wpc = ei("w_proj_c", [DEPTH, 256, D])
        self.wout = ei("w_out", [DEPTH, D, D])
        self.mwq = ei("mem_wq", [DEPTH, D, D])
        self.mwkv = ei("mem_wkv", [DEPTH, D, 2 * D])
        self.mwo = ei("mem_wo", [DEPTH, D, D])
        self.ln_g = ei("ln_g", [DEPTH, 3, D])
        self.ln_b = ei("ln_b", [DEPTH, 3, D])
        self.fgu = ei("ffn_w_gu", [2, D, 2 * DFF])
        self.fdn = ei("ffn_w_down", [2, DFF, D])
        self.mrt = ei("moe_router_t", [2, NE, D])
        self.mgu = ei("moe_w_gu", [2, NE, D, 2 * DFF])
        self.mdn = ei("moe_w_down", [2, NE, DFF, D])
        self.rope = ei("rope", [S, 96])
        self.out = nc.dram_tensor("out", [T, D], F32, kind="ExternalOutput").ap()
        it = lambda n, s, d=BF16: nc.dram_tensor(n, s, d, kind="Internal").ap()
        NT = self.NT
        self.H = it("H", [T, D], F32)
        self.HT = it("HT", [NT, 128, 8, 128])
        self.MEMT = it("MEMT", [128, 8, 256])
        self.AQT = it("AQT", [2, 128, S]); self.AKT = it("AKT", [2, 128, S])
        self.AV1 = it("AV1", [S, 4, 65])
        self.BQT = it("BQT", [4, 128, S]); self.BKT = it("BKT", [2, 128, S])
        self.BV1 = it("BV1", [S, 2, 65])
        self.CQT = it("CQT", [4, 96, S]); self.CKT = it("CKT", [4, 96, S])
        self.CV1 = it("CV1", [S, 4, 65])
        self.GT = it("GT", [24, 128, T])
        self.YT = it("YT", [16, 64, T])
        self.GATE = it("GATE", [T, NE], F32)
        self.bHt = [Buf("H%d" % i) for i in range(NT)]; self.bHTt = [Buf("HT%d" % i) for i in range(NT)]; self.bMEMT = Buf("MEMT")
        self.bA = Buf("attn_in"); self.bGT = Buf("GT"); self.bYT = Buf("YT"); self.bGATE = Buf("GATE")
        L = nlayers
        self.WINFM = it("WINFM", [L, 31, 128, 8, 128]); self.WINTM = it("WINTM", [L, 128, 8, 1440])
        self.WPA = it("WPA", [L, 128, 2, D]); self.WPB = it("WPB", [L, 128, 4, D]); self.WPC = it("WPC", [L, 128, 2, D])
        self.WOUT = it("WOUT", [L, 128, 8, D]); self.WQ = it("WQ", [L, 128, 8, D]); self.WKV = it("WKV", [L, 128, 8, 2 * D])
        self.WO = it("WO", [L, 128, 8, D])
        nE = sum(1 if l % 2 == 0 else NE for l in range(L))
        self.WGU = it("WGU", [nE, 22, 128, 8, 256]); self.WD = it("WD", [nE, 128, 22, D])
        self.bW = {}
        self.ebase = {}
        b = 0
        for l in range(L):
            self.ebase[l] = b
            b += 1 if l % 2 == 0 else NE
        self.dbg = {}

    def cv(self, key, out, in_):
        f = self.f
        b = self.bW.get(key)
        if b is None:
            b = self.bW[key] = Buf(key)
            f.dma("pool", out, in_, writes=[b], pool="cv")
        else:
            f.dma("pool", out, in_, joins=[b], pool="cv")

    def convert_layer_attn(self, l):
        w = self.w_in[l].rearrange("(kc p) c -> p kc c", p=128)
        fm_cols = [0, 128, 256, 384, 1536, 1664, 1792] + [1952 + 128 * i for i in range(24)]
        self.cv(("wintm", l), self.WINTM[l], w[:, :, 512:1952])
        for m, c0 in enumerate(fm_cols):
            self.cv(("winfm", l, m), self.WINFM[l, m], w[:, :, c0:c0 + 128])
        r = lambda a: a.rearrange("(kc p) c -> p kc c", p=128)
        self.cv(("wpa", l), self.WPA[l], r(self.wpa[l]))
        self.cv(("wpb", l), self.WPB[l], r(self.wpb[l]))
        self.cv(("wpc", l), self.WPC[l], r(self.wpc[l]))
        self.cv(("wout", l), self.WOUT[l], r(self.wout[l]))
        self.cv(("wq", l), self.WQ[l], r(self.mwq[l]))
        self.cv(("wkv", l), self.WKV[l], r(self.mwkv[l]))
        self.cv(("wo", l), self.WO[l], r(self.mwo[l]))

    def convert_layer_ffn(self, l):
        ne = 1 if l % 2 == 0 else NE
        for e in range(ne):
            gu = self.fgu[l // 2] if l % 2 == 0 else self.mgu[l // 2, e]
            dn = self.fdn[l // 2] if l % 2 == 0 else self.mdn[l // 2, e]
            ei = self.ebase[l] + e
            g3 = gu.rearrange("(kc p) c -> p kc c", p=128)
            for c in range(22):
                self.cv(("wgu", ei, c), self.WGU[ei, c, :, :, 0:128], g3[:, :, c * 128:(c + 1) * 128])
                self.cv(("wgu", ei, c), self.WGU[ei, c, :, :, 128:256], g3[:, :, DFF + c * 128:DFF + (c + 1) * 128])
            self.cv(("wd", ei), self.WD[ei], dn.rearrange("(c p) n -> p c n", p=128))

    def setup_common(self):
        f, nc = self.f, self.nc
        self.pb = [f.ps("bank%d" % i, [128, 512], F32) for i in range(6)]
        self.bpb = [Buf("bank%d" % i, x=True) for i in range(6)]
        self.pT = [f.ps("bankTA", [128, 512], BF16), f.ps("bankTB", [128, 512], BF16)]
        self.bpt = [Buf("ptA", x=True), Buf("ptB", x=True)]
        self.ident = f.sb("ident", [128, 128], BF16); self.bid = Buf("ident")
        identf = f.sb("identf", [128, 128], F32); bidf = Buf()
        self.ones16 = f.sb("ones16", [128, 128], BF16); self.bones = Buf("ones")
        self.ones32 = f.sb("ones32", [128, 64], F32)
        f.op("pool", lambda: nc.gpsimd.memset(identf[:], 0.0), writes=[bidf])
        f.op("pool", lambda: nc.gpsimd.affine_select(out=identf[:], in_=identf[:], compare_op=ALU.not_equal, fill=1.0,
                                                     base=0, pattern=[[-1, 128]], channel_multiplier=1),
             reads=[bidf], writes=[bidf])
        f.op("dve", lambda: nc.vector.tensor_copy(out=self.ident[:], in_=identf[:]), reads=[bidf], writes=[self.bid])
        f.op("dve", lambda: nc.vector.memset(self.ones16[:], 1.0), writes=[self.bones])
        f.op("dve", lambda: nc.vector.memset(self.ones32[:], 1.0), writes=[self.bones])
        self.sel64 = f.sb("sel64", [128, 64], F32)
        f.op("dve", lambda: nc.vector.memset(self.sel64[:], 0.0), writes=[self.bones])
        f.op("dve", lambda: nc.vector.memset(self.sel64[64:65, :], 1.0), writes=[self.bones])
        self.epsc = f.sb("epsc", [128, 2], F32)
        f.op("dve", lambda: nc.vector.memset(self.epsc[:, 0:1], LN_EPS), writes=[self.bones])
        f.op("dve", lambda: nc.vector.memset(self.epsc[:, 1:2], RMS_EPS), writes=[self.bones])
        self.ln_h = [f.sb("ln_h%d" % i, [128, D], F32) for i in range(2)]; self.bln_h = [Buf(), Buf()]
        self.ln_t = f.sb("ln_t", [128, D], F32); self.bln_t = Buf()
        self.ln_y = [f.sb("ln_y%d" % i, [128, D], F32) for i in range(2)]; self.bln_y = [Buf(), Buf()]
        self.ln_y16 = f.sb("ln_y16", [128, D], BF16); self.bln_y16 = Buf()
        self.ln_yT = [f.sb("ln_yT%d" % i, [128, D], BF16) for i in range(2)]; self.bln_yT = [Buf(), Buf()]
        self.ln_st = f.sb("ln_st", [128, 12], F32); self.ln_mv = f.sb("ln_mv", [128, 4], F32); self.bln_s = Buf()
        self.lnG = f.sb("lnG", [128, D], F32); self.lnB = f.sb("lnB", [128, D], F32); self.blnGB = Buf()
        self.ln_i = 0

    def load_ln_params(self, g_ap, b_ap):
        f = self.f
        f.dma("sp", self.lnG[:], g_ap.partition_broadcast(128), writes=[self.blnGB])
        f.dma("sp", self.lnB[:], b_ap.partition_broadcast(128), joins=[self.blnGB])

    def ln_tile(self, gt, delta, delta_bufs, dst=None):
        f, nc = self.f, self.nc
        i = self.ln_i % 2
        self.ln_i += 1
        h, bh = self.ln_h[i], self.bln_h[i]
        rows = slice(gt * 128, (gt + 1) * 128)
        if delta is None:
            f.dma("sp", h[:], self.x[rows, :], writes=[bh])
            t = h
            bt = bh
        else:
            f.dma("sp", h[:], self.H[rows, :], reads=[self.bHt[gt]], writes=[bh])
            t, bt = self.ln_t, self.bln_t
            for hf in range(2):
                cs = slice(hf * 512, (hf + 1) * 512)
                kw = dict(writes=[bt]) if hf == 0 else dict(joins=[bt])
                f.op("dve", lambda: nc.vector.scalar_tensor_tensor(out=t[:, cs], in0=h[:, cs], scalar=ALPHA, in1=delta[hf],
                                                                    op0=ALU.mult, op1=ALU.add),
                     reads=[bh, delta_bufs[hf]], **kw)
        import os
        CUT = int(os.environ.get("CUT", "99"))
        if CUT <= 0:
            return
        st, mv, bs = self.ln_st, self.ln_mv, self.bln_s
        f.op("dve", lambda: nc.vector.bn_stats(out=st[:, 0:6], in_=t[:, 0:512]), reads=[bt], writes=[bs])
        f.op("dve", lambda: nc.vector.bn_stats(out=st[:, 6:12], in_=t[:, 512:1024]), reads=[bt], joins=[bs])
        f.op("dve", lambda: nc.vector.bn_aggr(out=mv[:, 0:2], in_=st[:].rearrange("p (a b) -> p a b", b=6)), reads=[bs], joins=[bs])
        f.op("act", lambda: nc.scalar.activation(out=mv[:, 3:4], in_=mv[:, 1:2], func=AF.Sqrt, bias=self.epsc[:, 0:1], scale=1.0), reads=[bs], joins=[bs])
        f.op("dve", lambda: nc.vector.reciprocal(out=mv[:, 2:3], in_=mv[:, 3:4]), reads=[bs], joins=[bs])
        if CUT <= 1:
            return
        y, by = self.ln_y[i], self.bln_y[i]
        f.op("dve", lambda: nc.vector.tensor_scalar(out=y[:], in0=t[:], scalar1=mv[:, 0:1], scalar2=mv[:, 2:3],
                                                    op0=ALU.subtract, op1=ALU.mult), reads=[bt, bs], writes=[by])
        f.op("dve", lambda: nc.vector.tensor_mul(out=y[:], in0=y[:], in1=self.lnG[:]), reads=[by, self.blnGB], writes=[by])
        f.op("dve", lambda: nc.vector.tensor_add(out=y[:], in0=y[:], in1=self.lnB[:]), reads=[by, self.blnGB], writes=[by])
        if dst is not None:
            f.dma("act", dst[rows, :], y[:], reads=[by], joins=[self.bout])
            return
        if CUT <= 2:
            return
        f.dma("act", self.H[rows, :], y[:], reads=[by], writes=[self.bHt[gt]])
        if CUT <= 3:
            return
        y16, by16 = self.ln_y16, self.bln_y16
        f.op("act", lambda: nc.scalar.copy(out=y16[:], in_=y[:]), reads=[by], writes=[by16])
        yT, byT = self.ln_yT[i], self.bln_yT[i]
        for half in range(2):
            bp = self.bpt[half]
            for j in range(4):
                c = half * 4 + j
                kw = dict(writes=[bp]) if j == 0 else dict(joins=[bp])
                f.op("pe", lambda: nc.tensor.transpose(out=self.pT[half][:, j * 128:(j + 1) * 128],
                                                       in_=y16[:, c * 128:(c + 1) * 128], identity=self.ident[:]),
                     reads=[by16, self.bid], **kw)
            kw = dict(writes=[byT]) if half == 0 else dict(joins=[byT])
            if half == 0:
                f.op("dve", lambda: nc.vector.tensor_copy(out=yT[:, 0:512], in_=self.pT[0][:]), reads=[bp], **kw)
            else:
                f.op("act", lambda: nc.scalar.copy(out=yT[:, 512:1024], in_=self.pT[1][:]), reads=[bp], **kw)
        f.dma("act", self.HT[gt], yT[:].rearrange("p (c t) -> p c t", c=8), reads=[byT], writes=[self.bHTt[gt]])

    def new_gen(self, *bufs):
        for b in bufs:
            b.w = {}; b.r = {}; b.prev = {}

    def load_hT(self, dst, bdst, sb, ntile):
        f = self.f
        t0 = sb * ntile
        for n in range(ntile):
            kw = dict(writes=[bdst]) if n == 0 else dict(joins=[bdst])
            f.dma("sp", dst[:, :, n * 128:(n + 1) * 128], self.HT[t0 + n], reads=[self.bHTt[t0 + n]], **kw)

    def phase_embed(self):
        f, nc = self.f, self.nc
        self.load_ln_params(self.emb_g[0], self.emb_b[0])
        for gt in range(self.NT):
            self.ln_tile(gt, None, None)
        import os
        if os.environ.get("SKIP_MEM"):
            return
        f.push()
        m32 = f.sb("m32", [128, D], F32); bm32 = Buf()
        m16 = f.sb("m16", [128, D], BF16); bm16 = Buf()
        mT = f.sb("mT", [128, 8, 256], BF16); bmT = Buf()
        for mt in range(2):
            f.dma("sp", m32[:], self.mem[mt * 128:(mt + 1) * 128, :], writes=[bm32])
            f.op("act", lambda: nc.scalar.copy(out=m16[:], in_=m32[:]), reads=[bm32], writes=[bm16])
            for half in range(2):
                bp = self.bpt[half]
                for j in range(4):
                    c = half * 4 + j
                    kw = dict(writes=[bp]) if j == 0 else dict(joins=[bp])
                    f.op("pe", lambda: nc.tensor.transpose(out=self.pT[half][:, j * 128:(j + 1) * 128],
                                                           in_=m16[:, c * 128:(c + 1) * 128], identity=self.ident[:]),
                         reads=[bm16, self.bid], **kw)
                kw = dict(writes=[bmT]) if (mt == 0 and half == 0) else dict(joins=[bmT])
                f.op("dve", lambda: nc.vector.tensor_copy(
                    out=mT[:, half * 4:(half + 1) * 4, mt * 128:(mt + 1) * 128],
                    in_=self.pT[half][:].rearrange("p (c t) -> p c t", c=4)), reads=[bp], **kw)
        f.dma("sp", self.MEMT, mT[:], reads=[bmT], writes=[self.bMEMT])
        f.barrier()
        f.pop()

    def phase_A(self, l):
        f, nc = self.f, self.nc
        T = self.T
        f.push()
        wtm = f.sb("wtm", [128, 8, 1440], BF16); bwtm = Buf()
        f.dma("sp", wtm[:], self.WINTM[l], reads=[self.bW[("wintm", l)]], writes=[bwtm])
        wq32 = f.sb("wq32", [128, 2, 384], F32); wk32 = f.sb("wk32", [128, 512], F32); bw32 = Buf()
        gq = f.sb("gq", [128, 3], F32)
        wq16 = f.sb("wq16", [128, 2, 384], BF16); wk16 = f.sb("wk16", [128, 512], BF16); bw16 = Buf()
        f.dma("sp", wq32[:], self.wuq[l].rearrange("(kc p) c -> p kc c", p=128), writes=[bw32])
        f.dma("sp", wk32[:], self.wukv[l], joins=[bw32])
        f.dma("sp", gq[:, 0:2], self.mqn[l].rearrange("(kc p) -> p kc", p=128), joins=[bw32], allow_slow_non_contiguous=True)
        f.dma("sp", gq[:, 2:3], self.mkn[l].rearrange("(kc p) -> p kc", p=128), joins=[bw32], allow_slow_non_contiguous=True)
        for kc in range(2):
            kw = dict(writes=[bw16]) if kc == 0 else dict(joins=[bw16])
            f.op("dve", lambda: nc.vector.tensor_scalar(out=wq16[:, kc, :], in0=wq32[:, kc, :], scalar1=gq[:, kc:kc + 1], scalar2=None,
                                                        op0=ALU.mult), reads=[bw32], **kw)
        f.op("dve", lambda: nc.vector.tensor_scalar(out=wk16[:], in0=wk32[:], scalar1=gq[:, 2:3], scalar2=None, op0=ALU.mult),
             reads=[bw32], joins=[bw16])
        gqk = f.sb("gqk", [128, 10, 64], F32); bgqk = Buf()
        for i in range(10):
            src = (self.gqn if i < 8 else self.gkn)[l].partition_broadcast(128)
            kw = dict(writes=[bgqk]) if i == 0 else dict(joins=[bgqk])
            f.dma("sp", gqk[:, i, :], src, **kw)
        hT = f.sb("hT", [128, 8, 1024], BF16); bhT = Buf()
        cdT = f.sb("cdT", [128, 3, 1024], BF16); bcdT = Buf()
        wfm = [f.sb("wfm%d" % i, [128, 8, 128], BF16) for i in range(3)]; bwfm = [Buf() for _ in range(3)]
        z = f.sb("z", [128, 1440], F32); bz = Buf()
        rp = f.sb("rp", [128, 96], F32); brp = Buf()
        sq = f.sb("sq", [128, 640], F32); bsq = Buf()
        ss = f.sb("ss", [128, 16], F32); bss = Buf()
        xn = f.sb("xn", [128, 640], F32); bxn = Buf()
        tA = f.sb("tA", [128, 320], F32); tB = f.sb("tB", [128, 320], F32); btAB = Buf()
        qk16 = f.sb("qk16", [128, 768], BF16); bqk16 = Buf()
        qcf = f.sb("qcf", [128, 384], F32); kvf = f.sb("kvf", [128, 512], F32); bqcf = Buf()
        kpe = f.sb("kpe", [128, 32], F32); bkpe = Buf()
        qf16 = f.sb("qf16", [128, 4, 96], BF16); kf16 = f.sb("kf16", [128, 4, 96], BF16); bf16b = Buf()
        junk = f.sb("junk", [128, 256], F32); bjunk = Buf()
        qT_sb = f.sb("qT_sb", [128, 4, 1024], BF16); kT_sb = f.sb("kT_sb", [128, 2, 1024], BF16)
        cqT_sb = f.sb("cqT_sb", [96, 4, 1024], BF16); ckT_sb = f.sb("ckT_sb", [96, 4, 1024], BF16); bT_sb = Buf()
        av1 = f.sb("av1", [128, 8, 4, 65], BF16); bv1 = f.sb("bv1", [128, 8, 2, 65], BF16); cv1 = f.sb("cv1", [128, 8, 4, 65], BF16)
        bv_sb = Buf()
        ev16 = [f.sb("ev16_%d" % i, [128, 512], BF16) for i in range(3)]; bev16 = [Buf() for _ in range(3)]
        f.op("dve", lambda: nc.vector.memset(av1[:], 1.0), writes=[bv_sb])
        f.op("dve", lambda: nc.vector.memset(bv1[:], 1.0), joins=[bv_sb])
        f.op("dve", lambda: nc.vector.memset(cv1[:], 1.0), joins=[bv_sb])
        self.new_gen(self.bA, self.bGT)
        nsb = T // 1024
        evi = 0
        fmi = 0
        for sb in range(nsb):
            self.load_hT(hT, bhT, sb, 8)
            tok0 = sb * 1024

            def fm_chunk(m, consume):
                nonlocal fmi
                w, bw = wfm[fmi % 3], bwfm[fmi % 3]
                fmi += 1
                f.dma("sp", w[:], self.WINFM[l, m], reads=[self.bW[("winfm", l, m)]], writes=[bw])
                for tb in range(2):
                    bank = (fmi * 2 + tb) % 3
                    for kc in range(8):
                        kw = dict(writes=[self.bpb[bank]]) if kc == 0 else dict(joins=[self.bpb[bank]])
                        f.op("pe", lambda: nc.tensor.matmul(self.pb[bank][:], lhsT=w[:, kc, :], rhs=hT[:, kc, tb * 512:(tb + 1) * 512],
                                                            start=(kc == 0), stop=(kc == 7)), reads=[bw, bhT], **kw)
                    consume(tb, bank)

            for i, m in enumerate((4, 5, 6)):
                def cons(tb, bank, i=i):
                    kw = dict(writes=[bcdT]) if (i == 0 and tb == 0) else dict(joins=[bcdT])
                    f.op("act", lambda: nc.scalar.copy(out=cdT[:, i, tb * 512:(tb + 1) * 512], in_=self.pb[bank][:]),
                         reads=[self.bpb[bank]], **kw)
                fm_chunk(m, cons)
            first_sbw = True
            for tt in range(8):
                gt = sb * 8 + tt
                tsl = slice(tt * 128, (tt + 1) * 128)
                for n, (c0, cw) in enumerate(((0, 512), (512, 512), (1024, 416))):
                    bank = 3 + n
                    for kc in range(8):
                        kw = dict(writes=[self.bpb[bank]]) if kc == 0 else dict(joins=[self.bpb[bank]])
                        f.op("pe", lambda: nc.tensor.matmul(self.pb[bank][:, 0:cw], lhsT=hT[:, kc, tsl], rhs=wtm[:, kc, c0:c0 + cw],
                                                            start=(kc == 0), stop=(kc == 7)), reads=[bhT, bwtm], **kw)
                    kw = dict(writes=[bz]) if n == 0 else dict(joins=[bz])
                    if n == 1:
                        f.op("dve", lambda: nc.vector.tensor_copy(out=z[:, c0:c0 + cw], in_=self.pb[bank][:, 0:cw]), reads=[self.bpb[bank]], **kw)
                    else:
                        f.op("act", lambda: nc.scalar.copy(out=z[:, c0:c0 + cw], in_=self.pb[bank][:, 0:cw]), reads=[self.bpb[bank]], **kw)
                f.dma("sp", rp[:], self.rope[gt * 128:(gt + 1) * 128, :], writes=[brp])
                kwv = dict(writes=[bv_sb]) if first_sbw else dict(joins=[bv_sb])
                kwT = dict(writes=[bT_sb]) if first_sbw else dict(joins=[bT_sb])
                first_sbw = False
                f.op("act", lambda: nc.scalar.copy(out=av1[:, tt, :, 0:64], in_=z[:, 0:256].rearrange("p (h d) -> p h d", h=4)), reads=[bz], **kwv)
                f.op("act", lambda: nc.scalar.copy(out=bv1[:, tt, :, 0:64], in_=z[:, 896:1024].rearrange("p (h d) -> p h d", h=2)), reads=[bz], joins=[bv_sb])
                zqk = z[:, 256:896]
                f.op("dve", lambda: nc.vector.tensor_tensor(out=sq[:], in0=zqk, in1=zqk, op=ALU.mult), reads=[bz], writes=[bsq])
                f.op("dve", lambda: nc.vector.tensor_reduce(out=ss[:, 0:10], in_=sq[:].rearrange("p (h d) -> p h d", h=10), axis=AX.X, op=ALU.add),
                     reads=[bsq], writes=[bss])
                f.op("act", lambda: nc.scalar.activation(out=ss[:, 0:10], in_=ss[:, 0:10], func=AF.Sqrt, bias=self.epsc[:, 1:2], scale=1.0 / 64),
                     reads=[bss], writes=[bss])
                f.op("dve", lambda: nc.vector.reciprocal(out=ss[:, 0:10], in_=ss[:, 0:10]), reads=[bss], writes=[bss])
                xn3 = xn[:].rearrange("p (h d) -> p h d", h=10)
                f.op("dve", lambda: nc.vector.tensor_tensor(out=xn3, in0=zqk.rearrange("p (h d) -> p h d", h=10),
                                                            in1=ss[:, 0:10].unsqueeze(2).broadcast_to([128, 10, 64]), op=ALU.mult),
                     reads=[bz, bss], writes=[bxn])
                f.op("dve", lambda: nc.vector.tensor_tensor(out=xn3, in0=xn3, in1=gqk[:], op=ALU.mult), reads=[bxn, bgqk], writes=[bxn])
                x1, x2 = xn3[:, :, 0:32], xn3[:, :, 32:64]
                cb = rp[:, 0:32].unsqueeze(1).broadcast_to([128, 10, 32]); sbn = rp[:, 32:64].unsqueeze(1).broadcast_to([128, 10, 32])
                tA3 = tA[:].rearrange("p (h d) -> p h d", h=10); tB3 = tB[:].rearrange("p (h d) -> p h d", h=10)
                q16 = qk16[:, 0:512].rearrange("p (h d) -> p h d", h=8)
                k16 = qk16[:, 512:768].rearrange("p (k r d) -> p k r d", k=2, r=2)
                f.op("dve", lambda: nc.vector.tensor_tensor(out=tA3, in0=x1, in1=cb, op=ALU.mult), reads=[bxn, brp], writes=[btAB])
                f.op("dve", lambda: nc.vector.tensor_tensor(out=tB3, in0=x2, in1=sbn, op=ALU.mult), reads=[bxn, brp], joins=[btAB])
                f.op("dve", lambda: nc.vector.tensor_tensor(out=q16[:, :, 0:32], in0=tA3[:, 0:8, :], in1=tB3[:, 0:8, :], op=ALU.subtract),
                     reads=[btAB], writes=[bqk16])
                for r in range(2):
                    f.op("dve", lambda: nc.vector.tensor_tensor(out=k16[:, :, r, 0:32], in0=tA3[:, 8:10, :], in1=tB3[:, 8:10, :], op=ALU.subtract),
                         reads=[btAB], joins=[bqk16])
                f.op("dve", lambda: nc.vector.tensor_tensor(out=tA3, in0=x2, in1=cb, op=ALU.mult), reads=[bxn, brp, bqk16], writes=[btAB])
                f.op("dve", lambda: nc.vector.tensor_tensor(out=tB3, in0=x1, in1=sbn, op=ALU.mult), reads=[bxn, brp], joins=[btAB])
                f.op("dve", lambda: nc.vector.tensor_tensor(out=q16[:, :, 32:64], in0=tA3[:, 0:8, :], in1=tB3[:, 0:8, :], op=ALU.add),
                     reads=[btAB], joins=[bqk16])
                for r in range(2):
                    f.op("dve", lambda: nc.vector.tensor_tensor(out=k16[:, :, r, 32:64], in0=tA3[:, 8:10, :], in1=tB3[:, 8:10, :], op=ALU.add),
                         reads=[btAB], joins=[bqk16])
                for j in range(4):
                    kw = dict(writes=[self.bpt[0]]) if j == 0 else dict(joins=[self.bpt[0]])
                    f.op("pe", lambda: nc.tensor.transpose(out=self.pT[0][:, j * 128:(j + 1) * 128], in_=qk16[:, j * 128:(j + 1) * 128], identity=self.ident[:]),
                         reads=[bqk16, self.bid], **kw)
                f.op("act", lambda: nc.scalar.copy(out=qT_sb[:, :, tsl], in_=self.pT[0][:].rearrange("p (c t) -> p c t", c=4)),
                     reads=[self.bpt[0]], **kwT)
                for j in range(2):
                    kw = dict(writes=[self.bpt[1]]) if j == 0 else dict(joins=[self.bpt[1]])
                    f.op("pe", lambda: nc.tensor.transpose(out=self.pT[1][:, j * 128:(j + 1) * 128], in_=qk16[:, 512 + j * 128:512 + (j + 1) * 128],
                                                           identity=self.ident[:]), reads=[bqk16, self.bid], **kw)
                f.op("act", lambda: nc.scalar.copy(out=kT_sb[:, :, tsl], in_=self.pT[1][:, 0:256].rearrange("p (c t) -> p c t", c=2)),
                     reads=[self.bpt[1]], joins=[bT_sb])
                f.op("act", lambda: nc.scalar.activation(out=junk[:, 0:256], in_=z[:, 1024:1280], func=AF.Square, accum_out=ss[:, 10:11]),
                     reads=[bz, bss], writes=[bjunk], joins=[bss])
                f.op("act", lambda: nc.scalar.activation(out=junk[:, 0:128], in_=z[:, 1280:1408], func=AF.Square, accum_out=ss[:, 11:12]),
                     reads=[bz], writes=[bjunk], joins=[bss])
                f.op("act", lambda: nc.scalar.activation(out=ss[:, 10:11], in_=ss[:, 10:11], func=AF.Sqrt, bias=self.epsc[:, 1:2], scale=1.0 / 256),
                     reads=[bss], writes=[bss])
                f.op("act", lambda: nc.scalar.activation(out=ss[:, 11:12], in_=ss[:, 11:12], func=AF.Sqrt, bias=self.epsc[:, 1:2], scale=1.0 / 128),
                     reads=[bss], writes=[bss])
                f.op("dve", lambda: nc.vector.reciprocal(out=ss[:, 10:12], in_=ss[:, 10:12]), reads=[bss], writes=[bss])
                for kc in range(2):
                    kw = dict(writes=[self.bpb[0]]) if kc == 0 else dict(joins=[self.bpb[0]])
                    f.op("pe", lambda: nc.tensor.matmul(self.pb[0][:, 0:384], lhsT=cdT[:, kc, tsl], rhs=wq16[:, kc, :], start=(kc == 0), stop=(kc == 1)),
                         reads=[bcdT, bw16], **kw)
                f.op("pe", lambda: nc.tensor.matmul(self.pb[1][:, 0:512], lhsT=cdT[:, 2, tsl], rhs=wk16[:], start=True, stop=True),
                     reads=[bcdT, bw16], writes=[self.bpb[1]])
                f.op("dve", lambda: nc.vector.tensor_scalar(out=qcf[:], in0=self.pb[0][:, 0:384], scalar1=ss[:, 10:11], scalar2=None, op0=ALU.mult),
                     reads=[self.bpb[0], bss], writes=[bqcf])
                f.op("dve", lambda: nc.vector.tensor_scalar(out=kvf[:], in0=self.pb[1][:, 0:512], scalar1=ss[:, 11:12], scalar2=None, op0=ALU.mult),
                     reads=[self.bpb[1], bss], joins=[bqcf])
                qc3 = qcf[:].rearrange("p (h d) -> p h d", h=4)
                kv3 = kvf[:].rearrange("p (h d) -> p h d", h=4)
                cc = rp[:, 64:80]; sc = rp[:, 80:96]
                cc4 = cc.unsqueeze(1).broadcast_to([128, 4, 16]); sc4 = sc.unsqueeze(1).broadcast_to([128, 4, 16])
                tq = tA[:, 0:64].rearrange("p (h d) -> p h d", h=4); tq2 = tB[:, 0:64].rearrange("p (h d) -> p h d", h=4)
                q1, q2 = qc3[:, :, 64:80], qc3[:, :, 80:96]
                f.op("act", lambda: nc.scalar.copy(out=qf16[:, :, 0:64], in_=qc3[:, :, 0:64]), reads=[bqcf], writes=[bf16b])
                f.op("dve", lambda: nc.vector.tensor_tensor(out=tq, in0=q1, in1=cc4, op=ALU.mult), reads=[bqcf, brp, bqk16], writes=[btAB])
                f.op("dve", lambda: nc.vector.tensor_tensor(out=tq2, in0=q2, in1=sc4, op=ALU.mult), reads=[bqcf, brp], joins=[btAB])
                f.op("dve", lambda: nc.vector.tensor_tensor(out=qf16[:, :, 64:80], in0=tq, in1=tq2, op=ALU.subtract), reads=[btAB], joins=[bf16b])
                f.op("dve", lambda: nc.vector.tensor_tensor(out=tq, in0=q2, in1=cc4, op=ALU.mult), reads=[bqcf, brp, bf16b], writes=[btAB])
                f.op("dve", lambda: nc.vector.tensor_tensor(out=tq2, in0=q1, in1=sc4, op=ALU.mult), reads=[bqcf, brp], joins=[btAB])
                f.op("dve", lambda: nc.vector.tensor_tensor(out=qf16[:, :, 80:96], in0=tq, in1=tq2, op=ALU.add), reads=[btAB], joins=[bf16b])
                k1, k2 = z[:, 1408:1424], z[:, 1424:1440]
                t1, t2 = tA[:, 64:80], tB[:, 64:80]
                f.op("dve", lambda: nc.vector.tensor_tensor(out=t1, in0=k1, in1=cc, op=ALU.mult), reads=[bz, brp, bf16b], writes=[btAB])
                f.op("dve", lambda: nc.vector.tensor_tensor(out=t2, in0=k2, in1=sc, op=ALU.mult), reads=[bz, brp], joins=[btAB])
                f.op("dve", lambda: nc.vector.tensor_tensor(out=kpe[:, 0:16], in0=t1, in1=t2, op=ALU.subtract), reads=[btAB], writes=[bkpe])
                f.op("dve", lambda: nc.vector.tensor_tensor(out=t1, in0=k2, in1=cc, op=ALU.mult), reads=[bz, brp, bkpe], writes=[btAB])
                f.op("dve", lambda: nc.vector.tensor_tensor(out=t2, in0=k1, in1=sc, op=ALU.mult), reads=[bz, brp], joins=[btAB])
                f.op("dve", lambda: nc.vector.tensor_tensor(out=kpe[:, 16:32], in0=t1, in1=t2, op=ALU.add), reads=[btAB], joins=[bkpe])
                f.op("act", lambda: nc.scalar.copy(out=kf16[:, :, 0:64], in_=kv3[:, :, 0:64]), reads=[bqcf], joins=[bf16b])
                f.op("dve", lambda: nc.vector.tensor_copy(out=kf16[:, :, 64:96], in_=kpe[:].unsqueeze(1).broadcast_to([128, 4, 32])),
                     reads=[bkpe], joins=[bf16b])
                f.op("act", lambda: nc.scalar.copy(out=cv1[:, tt, :, 0:64], in_=kv3[:, :, 64:128]), reads=[bqcf], joins=[bv_sb])
                for which, (src16, dstT) in enumerate(((qf16, cqT_sb), (kf16, ckT_sb))):
                    bp = self.bpt[which]
                    for h in range(4):
                        kw = dict(writes=[bp]) if h == 0 else dict(joins=[bp])
                        f.op("pe", lambda: nc.tensor.transpose(out=self.pT[which][0:96, h * 128:(h + 1) * 128],
                                                               in_=src16[:, h, :], identity=self.ident[:]), reads=[bf16b, self.bid], **kw)
                    f.op("dve" if which == 0 else "act",
                         (lambda: nc.vector.tensor_copy(out=dstT[:, :, tsl], in_=self.pT[which][0:96, :].rearrange("p (c t) -> p c t", c=4)))
                         if which == 0 else
                         (lambda: nc.scalar.copy(out=dstT[:, :, tsl], in_=self.pT[which][0:96, :].rearrange("p (c t) -> p c t", c=4))),
                         reads=[bp], joins=[bT_sb])
            tsb = slice(tok0, tok0 + 1024)
            f.dma("act", self.BQT[:, :, tsb].rearrange("c p t -> p c t"), qT_sb[:], reads=[bT_sb], joins=[self.bA])
            f.dma("act", self.BKT[:, :, tsb].rearrange("c p t -> p c t"), kT_sb[:], reads=[bT_sb], joins=[self.bA])
            f.dma("act", self.CQT[:, :, tsb].rearrange("c p t -> p c t"), cqT_sb[:], reads=[bT_sb], joins=[self.bA])
            f.dma("act", self.CKT[:, :, tsb].rearrange("c p t -> p c t"), ckT_sb[:], reads=[bT_sb], joins=[self.bA])
            f.dma("act", self.AV1[tsb].rearrange("(n p) h d -> p n h d", p=128), av1[:], reads=[bv_sb], joins=[self.bA])
            f.dma("act", self.BV1[tsb].rearrange("(n p) h d -> p n h d", p=128), bv1[:], reads=[bv_sb], joins=[self.bA])
            f.dma("act", self.CV1[tsb].rearrange("(n p) h d -> p n h d", p=128), cv1[:], reads=[bv_sb], joins=[self.bA])
            for m in (0, 1, 2, 3):
                def cons(tb, bank, m=m):
                    nonlocal evi
                    e16, be = ev16[evi % 3], bev16[evi % 3]
                    evi += 1
                    f.op("dve", lambda: nc.vector.tensor_copy(out=e16[:], in_=self.pb[bank][:]), reads=[self.bpb[bank]], writes=[be])
                    dst = (self.AQT if m < 2 else self.AKT)[m % 2, :, tok0 + tb * 512: tok0 + (tb + 1) * 512]
                    f.dma("act", dst, e16[:], reads=[be], joins=[self.bA])
                fm_chunk(m, cons)
            for g in range(24):
                def cons(tb, bank, g=g):
                    nonlocal evi
                    e16, be = ev16[evi % 3], bev16[evi % 3]
                    evi += 1
                    f.op("act", lambda: nc.scalar.activation(out=e16[:], in_=self.pb[bank][:], func=AF.Sigmoid), reads=[self.bpb[bank]], writes=[be])
                    f.dma("act", self.GT[g, :, tok0 + tb * 512: tok0 + (tb + 1) * 512], e16[:], reads=[be], joins=[self.bGT])
                fm_chunk(7 + g, cons)
        f.barrier()
        f.pop()

    def full_attn(self, l, QT, KT_of, V1, nheads, kdim, scale, y0, kv_of, qsel):
        f, nc = self.f, self.nc
        T = self.T
        f.push()
        kT = f.sb("kT", [128, S], BF16); bkT = Buf()
        v1 = f.sb("v1", [128, 32, 65], BF16); bv = Buf()
        qT = [f.sb("qT%d" % i, [128, 512], BF16) for i in range(4)]; bq = [Buf() for _ in range(4)]
        for i in range(4):
            f.op("dve", lambda: nc.vector.memset(qT[i][:], 0.0), writes=[bq[i]])
        pT = [f.sb("pT%d" % i, [128, 512], BF16) for i in range(3)]; bp = [Buf() for _ in range(3)]
        rc = f.sb("rc", [128, 512], F32); brc = Buf()
        f.op("dve", lambda: nc.vector.memset(rc[:], 0.0), writes=[brc])
        fullk = (kdim == 64)
        kps_mm = slice(0, 128) if fullk else slice(0, kdim)
        rb = f.sb("rb", [64, 512], F32); brb = Buf()
        yo = [f.sb("yo%d" % i, [64, 512], BF16) for i in range(2)]; byo = [Buf(), Buf()]
        nqb = T // 512
        cur_kv = None
        it = 0
        for h in range(nheads):
            kv = kv_of(h)
            qc, ps = qsel(h)
            if kv != cur_kv:
                cur_kv = kv
                ksrc, kps = KT_of(kv)
                f.dma("sp", kT[kps, :], ksrc, reads=[self.bA], writes=[bkT])
                f.dma("sp", v1[:], V1[:, kv, :].rearrange("(n p) d -> p n d", p=128), reads=[self.bA], writes=[bv])
            for qb in range(nqb):
                def q_load(h_, qb_, it_):
                    qc_, ps_ = qsel(h_)
                    qi_ = (it_ % 2) + (2 if ps_.start else 0)
                    f.dma("sp", qT[qi_][ps_, :], QT[qc_, ps_, qb_ * 512:(qb_ + 1) * 512], reads=[self.bA], writes=[bq[qi_]])
                if it == 0:
                    q_load(h, qb, it)
                nh, nqb_ = (h, qb + 1) if qb + 1 < nqb else (h + 1, 0)
                if nh < nheads:
                    q_load(nh, nqb_, it + 1)
                qi = (it % 2) + (2 if ps.start else 0)
                q, bqq = qT[qi], bq[qi]
                obank = 4 + (it % 2)
                bo = self.bpb[obank]
                NK = S // 128

                def s_mm(kt):
                    bank = kt % 4
                    f.op("pe", lambda: nc.tensor.matmul(self.pb[bank][:], lhsT=kT[kps_mm, kt * 128:(kt + 1) * 128], rhs=q[kps_mm, :], start=True, stop=True),
                         reads=[bkT, bqq], writes=[self.bpb[bank]])
                    p, bpp = pT[kt % 3], bp[kt % 3]
                    f.op("act", lambda: nc.scalar.activation(out=p[:], in_=self.pb[bank][:], func=AF.Exp, scale=scale),
                         reads=[self.bpb[bank]], writes=[bpp])

                def pv_mm(kt):
                    p, bpp = pT[kt % 3], bp[kt % 3]
                    kw = dict(writes=[bo]) if kt == 0 else dict(joins=[bo])
                    f.op("pe", lambda: nc.tensor.matmul(self.pb[obank][0:65, :], lhsT=v1[:, kt, :], rhs=p[:], start=(kt == 0), stop=(kt == NK - 1)),
                         reads=[bv, bpp], **kw)
                s_mm(0)
                s_mm(1)
                for kt in range(NK):
                    if kt + 2 < NK:
                        s_mm(kt + 2)
                    pv_mm(kt)
                f.op("dve", lambda: nc.vector.reciprocal(out=rc[64:65, :], in_=self.pb[obank][64:65, :]), reads=[bo], writes=[brc])
                f.op("pe", lambda: nc.tensor.matmul(self.pb[3][0:64, :], lhsT=self.sel64[:], rhs=rc[:], start=True, stop=True),
                     reads=[brc, self.bones], writes=[self.bpb[3]])
                f.op("dve", lambda: nc.vector.tensor_copy(out=rb[:], in_=self.pb[3][0:64, :]), reads=[self.bpb[3]], writes=[brb])
                y, by = yo[it % 2], byo[it % 2]
                f.op("dve", lambda: nc.vector.tensor_tensor(out=y[:], in0=self.pb[obank][0:64, :], in1=rb[:], op=ALU.mult), reads=[bo, brb], writes=[by])
                f.dma("sp", self.YT[y0 + h, :, qb * 512:(qb + 1) * 512], y[:], reads=[by], joins=[self.bYT])
                it += 1
        f.barrier()
        f.pop()

    def na_attn(self, l):
        f, nc = self.f, self.nc
        T = self.T
        f.push()
        nbI = f.sb("nbI", [128, 5, 512], F32); bnbI = Buf()
        nbE = f.sb("nbE", [128, 5, 512], F32); bnbE = Buf()
        f.dma("sp", nbI[:], self.nab[l, 2].rearrange("t p c -> p t c"), writes=[bnbI])
        kT = f.sb("nkT", [128, 2, 640], BF16); bkT = [Buf(), Buf()]
        kTb = f.sb("nkTb", [128, 2, 640], BF16)
        v1 = [f.sb("nv1_%d" % i, [128, 5, 4, 65], BF16) for i in range(2)]; bv = [Buf(), Buf()]
        qT = [f.sb("nqT%d" % i, [128, 4, 128], BF16) for i in range(2)]; bq = [Buf(), Buf()]
        for i in range(2):
            f.op("dve", lambda: nc.vector.memset(qT[i][:], 0.0), writes=[bq[i]])
        sc = [f.sb("nsc%d" % i, [128, 512], F32) for i in range(2)]; bsc = [Buf(), Buf()]
        pT = [f.sb("npT%d" % i, [128, 512], BF16) for i in range(2)]; bp = [Buf(), Buf()]
        rc = f.sb("nrc", [128, 512], F32); brc = Buf()
        f.op("dve", lambda: nc.vector.memset(rc[:], 0.0), writes=[brc])
        rb = f.sb("nrb", [64, 512], F32); brb = Buf()
        yo = [f.sb("nyo%d" % i, [64, 4, 128], BF16) for i in range(2)]; byo = [Buf(), Buf()]
        kTs = [kT, kTb]
        nblk = T // 128
        pat_of = {0: 0, 1: 1, 30: 3, 31: 4}
        si = 0
        def na_loads(j):
            i2 = j % 2
            k0 = min(max(j - 2, 0), 27)
            f.dma("sp", kTs[i2][:], self.AKT[:, :, k0 * 128:(k0 + 5) * 128].rearrange("c p t -> p c t"), reads=[self.bA], writes=[bkT[i2]])
            f.dma("sp", v1[i2][:], self.AV1[k0 * 128:(k0 + 5) * 128].rearrange("(n p) h d -> p n h d", p=128), reads=[self.bA], writes=[bv[i2]])
            for hf in range(2):
                kwq = dict(writes=[bq[i2]]) if hf == 0 else dict(joins=[bq[i2]])
                f.dma("sp", qT[i2][hf * 64:(hf + 1) * 64, :, :].rearrange("p (c two) t -> p c two t", two=2)[:, :, hf, :],
                      self.AQT[:, hf * 64:(hf + 1) * 64, j * 128:(j + 1) * 128].rearrange("c p t -> p c t"), reads=[self.bA], **kwq)
        na_loads(0)
        for j in range(nblk):
            i2 = j % 2
            kt_, bk_ = kTs[i2], bkT[i2]
            if j + 1 < nblk:
                na_loads(j + 1)
            if j in pat_of:
                f.dma("sp", nbE[:], self.nab[l, pat_of[j]].rearrange("t p c -> p t c"), writes=[bnbE])
                nb, bnb = nbE, bnbE
            else:
                nb, bnb = nbI, bnbI
            for t in range(5):
                sbank = si % 2
                for h in range(4):
                    c = h // 2
                    kw = dict(writes=[self.bpb[sbank]]) if h == 0 else dict(joins=[self.bpb[sbank]])
                    f.op("pe", lambda: nc.tensor.matmul(self.pb[sbank][:, h * 128:(h + 1) * 128], lhsT=kt_[:, c, t * 128:(t + 1) * 128],
                                                        rhs=qT[i2][:, h, :], start=True, stop=True), reads=[bk_, bq[i2]], **kw)
                s_, bs_ = sc[si % 2], bsc[si % 2]
                p_, bp_ = pT[si % 2], bp[si % 2]
                f.op("dve", lambda: nc.vector.scalar_tensor_tensor(out=s_[:], in0=self.pb[sbank][:], scalar=0.125, in1=nb[:, t, :],
                                                                    op0=ALU.mult, op1=ALU.add), reads=[self.bpb[sbank], bnb], writes=[bs_])
                f.op("act", lambda: nc.scalar.activation(out=p_[:], in_=s_[:], func=AF.Exp), reads=[bs_], writes=[bp_])
                for h in range(4):
                    bo = self.bpb[2 + h]
                    kw = dict(writes=[bo]) if t == 0 else dict(joins=[bo])
                    f.op("pe", lambda: nc.tensor.matmul(self.pb[2 + h][0:65, 0:128], lhsT=v1[i2][:, t, h, :], rhs=p_[:, h * 128:(h + 1) * 128],
                                                        start=(t == 0), stop=(t == 4)), reads=[bv[i2], bp_], **kw)
                si += 1
            for h in range(4):
                kw = dict(writes=[brc]) if h == 0 else dict(joins=[brc])
                f.op("dve", lambda: nc.vector.reciprocal(out=rc[64:65, h * 128:(h + 1) * 128], in_=self.pb[2 + h][64:65, 0:128]),
                     reads=[self.bpb[2 + h]], **kw)
            f.op("pe", lambda: nc.tensor.matmul(self.pb[0][0:64, :], lhsT=self.sel64[:], rhs=rc[:], start=True, stop=True),
                 reads=[brc, self.bones], writes=[self.bpb[0]])
            f.op("act", lambda: nc.scalar.copy(out=rb[:], in_=self.pb[0][0:64, :]), reads=[self.bpb[0]], writes=[brb])
            y, by = yo[i2], byo[i2]
            for h in range(4):
                kw = dict(writes=[by]) if h == 0 else dict(joins=[by])
                f.op("dve", lambda: nc.vector.tensor_tensor(out=y[:, h, :], in0=self.pb[2 + h][0:64, 0:128], in1=rb[:, h * 128:(h + 1) * 128], op=ALU.mult),
                     reads=[self.bpb[2 + h], brb], **kw)
            f.dma("sp", self.YT[0:4, :, j * 128:(j + 1) * 128].rearrange("h p t -> p h t"), y[:], reads=[by], joins=[self.bYT])
        f.barrier()
        f.pop()

    def phase_B(self, l):
        import os
        parts = os.environ.get("BPARTS", "ngm")
        self.new_gen(self.bYT)
        if "n" in parts:
            self.na_attn(l)
        if "g" in parts:
            self.full_attn(l, self.BQT, lambda kv: (self.BKT[kv], slice(0, 128)), self.BV1, 8, 64, 0.125, 4,
                           lambda h: h // 4, lambda h: (h // 2, slice((h % 2) * 64, (h % 2) * 64 + 64)))
        if "m" in parts:
            self.full_attn(l, self.CQT, lambda kv: (self.CKT[kv], slice(0, 96)), self.CV1, 4, 96, float(96 ** -0.5), 12,
                           lambda h: h, lambda h: (h, slice(0, 96)))

    def out_proj_ln(self, srcT, bsrc, w, bw, gt, tt, nk):
        f, nc = self.f, self.nc
        banks = (4, 5)
        for hf in range(2):
            bank = banks[hf]
            for kc in range(nk):
                kw = dict(writes=[self.bpb[bank]]) if kc == 0 else dict(joins=[self.bpb[bank]])
                f.op("pe", lambda: nc.tensor.matmul(self.pb[bank][:], lhsT=srcT[:, kc, tt * 128:(tt + 1) * 128], rhs=w[:, kc, hf * 512:(hf + 1) * 512],
                                                    start=(kc == 0), stop=(kc == nk - 1)), reads=[bsrc, bw], **kw)
        self.ln_tile(gt, [self.pb[4][:], self.pb[5][:]], [self.bpb[4], self.bpb[5]])

    def phase_C(self, l):
        f, nc = self.f, self.nc
        T = self.T
        f.push()
        self.load_ln_params(self.ln_g[l, 0], self.ln_b[l, 0])
        wp = f.sb("wp", [128, 8, D], BF16); bwp = Buf()
        wo = f.sb("wo", [128, 8, D], BF16); bwo = Buf()
        f.dma("sp", wp[:, 0:2, :], self.WPA[l], reads=[self.bW[("wpa", l)]], writes=[bwp])
        f.dma("sp", wp[:, 2:6, :], self.WPB[l], reads=[self.bW[("wpb", l)]], joins=[bwp])
        f.dma("sp", wp[:, 6:8, :], self.WPC[l], reads=[self.bW[("wpc", l)]], joins=[bwp])
        f.dma("sp", wo[:], self.WOUT[l], reads=[self.bW[("wout", l)]], writes=[bwo])
        yT = [f.sb("yT%d" % i, [128, 8, 512], BF16) for i in range(2)]; byT = [Buf(), Buf()]
        gt_ = [f.sb("gts%d" % i, [128, 3, 512], BF16) for i in range(2)]; bgt = [Buf(), Buf()]
        mg = f.sb("mg", [128, 8, 512], BF16); bmg = Buf()
        t1 = f.sb("t1", [128, 512], F32); t2 = f.sb("t2", [128, 512], F32); bt = Buf()
        gi = 0
        for tb in range(T // 512):
            ts = slice(tb * 512, (tb + 1) * 512)
            y, by = yT[tb % 2], byT[tb % 2]
            for c in range(8):
                for two in range(2):
                    kw = dict(writes=[by]) if (c == 0 and two == 0) else dict(joins=[by])
                    f.dma("sp", y[two * 64:(two + 1) * 64, c, :], self.YT[2 * c + two, :, ts], reads=[self.bYT], **kw)
            for m in range(8):
                g, bg = gt_[gi % 2], bgt[gi % 2]
                gi += 1
                f.dma("sp", g[:], self.GT[:, :, ts].rearrange("(b m) p t -> m p b t", m=8)[m], reads=[self.bGT], writes=[bg])
                ms = slice(m * 128, (m + 1) * 128)
                for br, (k0, nk) in enumerate(((0, 2), (2, 4), (6, 2))):
                    bank = br
                    for kc in range(nk):
                        kw = dict(writes=[self.bpb[bank]]) if kc == 0 else dict(joins=[self.bpb[bank]])
                        f.op("pe", lambda: nc.tensor.matmul(self.pb[bank][:], lhsT=wp[:, k0 + kc, ms], rhs=y[:, k0 + kc, :],
                                                            start=(kc == 0), stop=(kc == nk - 1)), reads=[bwp, by], **kw)
                f.op("dve", lambda: nc.vector.tensor_tensor(out=t1[:], in0=self.pb[0][:], in1=g[:, 0, :], op=ALU.mult), reads=[self.bpb[0], bg], writes=[bt])
                f.op("dve", lambda: nc.vector.tensor_tensor(out=t2[:], in0=self.pb[1][:], in1=g[:, 1, :], op=ALU.mult), reads=[self.bpb[1], bg], joins=[bt])
                f.op("dve", lambda: nc.vector.tensor_tensor(out=t1[:], in0=t1[:], in1=t2[:], op=ALU.add), reads=[bt], writes=[bt])
                f.op("dve", lambda: nc.vector.tensor_tensor(out=t2[:], in0=self.pb[2][:], in1=g[:, 2, :], op=ALU.mult), reads=[self.bpb[2], bg, bt], writes=[bt])
                kw = dict(writes=[bmg]) if m == 0 else dict(joins=[bmg])
                f.op("dve", lambda: nc.vector.tensor_tensor(out=mg[:, m, :], in0=t1[:], in1=t2[:], op=ALU.add), reads=[bt], **kw)
            for tt in range(4):
                self.out_proj_ln(mg, bmg, wo, bwo, tb * 4 + tt, tt, 8)
        f.barrier()
        f.pop()

    def phase_D(self, l):
        f, nc = self.f, self.nc
        T = self.T
        f.push()
        self.load_ln_params(self.ln_g[l, 1], self.ln_b[l, 1])
        kmT = f.sb("kmT", [128, 8, 256], BF16); bkm = Buf()
        vm = f.sb("vm", [128, 2, D], BF16); bvm = Buf()
        f.push()
        wkv = f.sb("wkv", [128, 8, 2 * D], BF16); bwkv = Buf()
        memT = f.sb("memT", [128, 8, 256], BF16); bmemT = Buf()
        f.dma("sp", wkv[:], self.WKV[l], reads=[self.bW[("wkv", l)]], writes=[bwkv])
        f.dma("sp", memT[:], self.MEMT, reads=[self.bMEMT], writes=[bmemT])
        for m in range(8):
            bank = m % 4
            for kc in range(8):
                kw = dict(writes=[self.bpb[bank]]) if kc == 0 else dict(joins=[self.bpb[bank]])
                f.op("pe", lambda: nc.tensor.matmul(self.pb[bank][:, 0:256], lhsT=wkv[:, kc, m * 128:(m + 1) * 128], rhs=memT[:, kc, :],
                                                    start=(kc == 0), stop=(kc == 7)), reads=[bwkv, bmemT], **kw)
            kw = dict(writes=[bkm]) if m == 0 else dict(joins=[bkm])
            f.op("dve", lambda: nc.vector.tensor_copy(out=kmT[:, m, :], in_=self.pb[bank][:, 0:256]), reads=[self.bpb[bank]], **kw)
        for mt in range(2):
            for hf in range(2):
                bank = 4 + hf
                for kc in range(8):
                    kw = dict(writes=[self.bpb[bank]]) if kc == 0 else dict(joins=[self.bpb[bank]])
                    f.op("pe", lambda: nc.tensor.matmul(self.pb[bank][:], lhsT=memT[:, kc, mt * 128:(mt + 1) * 128],
                                                        rhs=wkv[:, kc, D + hf * 512: D + (hf + 1) * 512], start=(kc == 0), stop=(kc == 7)),
                         reads=[bwkv, bmemT], **kw)
                kw = dict(writes=[bvm]) if (mt == 0 and hf == 0) else dict(joins=[bvm])
                f.op("act", lambda: nc.scalar.copy(out=vm[:, mt, hf * 512:(hf + 1) * 512], in_=self.pb[bank][:]), reads=[self.bpb[bank]], **kw)
        f.barrier()
        f.pop()
        wq = f.sb("wq", [128, 8, D], BF16); bwq = Buf()
        wo = f.sb("wo", [128, 8, D], BF16); bwo = Buf()
        f.dma("sp", wq[:], self.WQ[l], reads=[self.bW[("wq", l)]], writes=[bwq])
        f.dma("sp", wo[:], self.WO[l], reads=[self.bW[("wo", l)]], writes=[bwo])
        hT = [f.sb("hT%d" % i, [128, 8, 512], BF16) for i in range(2)]; bhT = [Buf(), Buf()]
        qT = f.sb("qT", [128, 8, 512], BF16); bqT = Buf()
        oT = f.sb("oT", [128, 8, 512], BF16); boT = Buf()
        pT = [f.sb("pT%d" % i, [128, 2, 512], BF16) for i in range(2)]; bpT = [Buf(), Buf()]
        rb = f.sb("rb", [128, 512], F32); brb = Buf()
        ntb = T // 512
        self.load_hT(hT[0], bhT[0], 0, 4)
        for tb in range(ntb):
            h_, bh_ = hT[tb % 2], bhT[tb % 2]
            if tb + 1 < ntb:
                self.load_hT(hT[(tb + 1) % 2], bhT[(tb + 1) % 2], tb + 1, 4)
            for m in range(8):
                bank = m % 4
                for kc in range(8):
                    kw = dict(writes=[self.bpb[bank]]) if kc == 0 else dict(joins=[self.bpb[bank]])
                    f.op("pe", lambda: nc.tensor.matmul(self.pb[bank][:], lhsT=wq[:, kc, m * 128:(m + 1) * 128], rhs=h_[:, kc, :],
                                                        start=(kc == 0), stop=(kc == 7)), reads=[bwq, bh_], **kw)
                kw = dict(writes=[bqT]) if m == 0 else dict(joins=[bqT])
                if m % 2 == 0:
                    f.op("dve", lambda: nc.vector.tensor_copy(out=qT[:, m, :], in_=self.pb[bank][:]), reads=[self.bpb[bank]], **kw)
                else:
                    f.op("act", lambda: nc.scalar.copy(out=qT[:, m, :], in_=self.pb[bank][:]), reads=[self.bpb[bank]], **kw)
            for h in range(4):
                p_, bp_ = pT[h % 2], bpT[h % 2]
                for mt in range(2):
                    bank = mt
                    for dc in range(2):
                        kw = dict(writes=[self.bpb[bank]]) if dc == 0 else dict(joins=[self.bpb[bank]])
                        f.op("pe", lambda: nc.tensor.matmul(self.pb[bank][:], lhsT=kmT[:, 2 * h + dc, mt * 128:(mt + 1) * 128], rhs=qT[:, 2 * h + dc, :],
                                                            start=(dc == 0), stop=(dc == 1)), reads=[bkm, bqT], **kw)
                    kw = dict(writes=[bp_]) if mt == 0 else dict(joins=[bp_])
                    f.op("act", lambda: nc.scalar.activation(out=p_[:, mt, :], in_=self.pb[bank][:], func=AF.Exp, scale=1.0 / 16),
                         reads=[self.bpb[bank]], **kw)
                for mt in range(2):
                    kw = dict(writes=[self.bpb[4]]) if mt == 0 else dict(joins=[self.bpb[4]])
                    f.op("pe", lambda: nc.tensor.matmul(self.pb[4][:], lhsT=self.ones16[:], rhs=p_[:, mt, :], start=(mt == 0), stop=(mt == 1)),
                         reads=[self.bones, bp_], **kw)
                f.op("dve", lambda: nc.vector.reciprocal(out=rb[:], in_=self.pb[4][:]), reads=[self.bpb[4]], writes=[brb])
                for dc in range(2):
                    bank = 2 + dc
                    for mt in range(2):
                        kw = dict(writes=[self.bpb[bank]]) if mt == 0 else dict(joins=[self.bpb[bank]])
                        f.op("pe", lambda: nc.tensor.matmul(self.pb[bank][:], lhsT=vm[:, mt, h * 256 + dc * 128: h * 256 + (dc + 1) * 128], rhs=p_[:, mt, :],
                                                            start=(mt == 0), stop=(mt == 1)), reads=[bvm, bp_], **kw)
                    kw = dict(writes=[boT]) if (h == 0 and dc == 0) else dict(joins=[boT])
                    f.op("dve", lambda: nc.vector.tensor_tensor(out=oT[:, 2 * h + dc, :], in0=self.pb[bank][:], in1=rb[:], op=ALU.mult),
                         reads=[self.bpb[bank], brb], **kw)
            for tt in range(4):
                self.out_proj_ln(oT, boT, wo, bwo, tb * 4 + tt, tt, 8)
        f.barrier()
        f.pop()

    def phase_router(self, l):
        f, nc = self.f, self.nc
        f.push()
        wr = f.sb("wr", [128, NE, D], F32); bwr = Buf()
        f.dma("sp", wr[:].rearrange("p e d -> p (e d)"), self.mrt[l // 2].rearrange("e d -> (e d)").partition_broadcast(128), writes=[bwr])
        hb = [f.sb("hb%d" % i, [128, D], F32) for i in range(2)]; bhb = [Buf(), Buf()]
        pr = [f.sb("pr%d" % i, [128, D], F32) for i in range(2)]; bpr = [Buf(), Buf()]
        junk = f.sb("junk", [128, D], F32); bjunk = Buf()
        lg = [f.sb("lg%d" % i, [128, 32], F32) for i in range(2)]; blg = [Buf(), Buf()]
        self.new_gen(self.bGATE)
        pi = 0
        for gt in range(self.NT):
            h, bh = hb[gt % 2], bhb[gt % 2]
            g, bg = lg[gt % 2], blg[gt % 2]
            f.dma("sp", h[:], self.H[gt * 128:(gt + 1) * 128, :], reads=[self.bHt[gt]], writes=[bh])
            for e in range(NE):
                p, bp = pr[pi % 2], bpr[pi % 2]
                pi += 1
                f.op("dve", lambda: nc.vector.tensor_tensor(out=p[:], in0=h[:], in1=wr[:, e, :], op=ALU.mult), reads=[bh, bwr], writes=[bp])
                kw = dict(writes=[bjunk, bg]) if e == 0 else dict(writes=[bjunk], joins=[bg])
                f.op("act", lambda: nc.scalar.activation(out=junk[:], in_=p[:], func=AF.Identity, accum_out=g[:, e:e + 1]),
                     reads=[bp], **kw)
            L = g[:, 0:8]; m1 = g[:, 8:9]; m2 = g[:, 9:10]; k1 = g[:, 10:18]; L2 = g[:, 18:26]; w1 = g[:, 26:27]; w2 = g[:, 27:28]; d = g[:, 28:29]
            k2 = L2
            f.op("dve", lambda: nc.vector.tensor_reduce(out=m1, in_=L, axis=AX.X, op=ALU.max), reads=[bg], writes=[bg])
            f.op("dve", lambda: nc.vector.tensor_scalar(out=k1, in0=L, scalar1=m1, scalar2=None, op0=ALU.is_equal), reads=[bg], writes=[bg])
            f.op("dve", lambda: nc.vector.scalar_tensor_tensor(out=L2, in0=k1, scalar=-1e30, in1=L, op0=ALU.mult, op1=ALU.add), reads=[bg], writes=[bg])
            f.op("dve", lambda: nc.vector.tensor_reduce(out=m2, in_=L2, axis=AX.X, op=ALU.max), reads=[bg], writes=[bg])
            f.op("dve", lambda: nc.vector.tensor_scalar(out=k2, in0=L2, scalar1=m2, scalar2=None, op0=ALU.is_equal), reads=[bg], writes=[bg])
            f.op("dve", lambda: nc.vector.tensor_tensor(out=d, in0=m1, in1=m2, op=ALU.subtract), reads=[bg], writes=[bg])
            f.op("act", lambda: nc.scalar.activation(out=w1, in_=d, func=AF.Sigmoid), reads=[bg], writes=[bg])
            f.op("dve", lambda: nc.vector.tensor_scalar(out=w2, in0=w1, scalar1=-1.0, scalar2=1.0, op0=ALU.mult, op1=ALU.add), reads=[bg], writes=[bg])
            f.op("dve", lambda: nc.vector.tensor_scalar(out=k1, in0=k1, scalar1=w1, scalar2=None, op0=ALU.mult), reads=[bg], writes=[bg])
            f.op("dve", lambda: nc.vector.scalar_tensor_tensor(out=L, in0=k2, scalar=w2, in1=k1, op0=ALU.mult, op1=ALU.add), reads=[bg], writes=[bg])
            f.dma("sp", self.GATE[gt * 128:(gt + 1) * 128, :], L, reads=[bg], joins=[self.bGATE])
        f.barrier()
        f.pop()

    def phase_E(self, l, final):
        f, nc = self.f, self.nc
        T = self.T
        moe = (l % 2 == 1)
        ne = NE if moe else 1
        if moe:
            self.phase_router(l)
        f.push()
        self.load_ln_params(self.ln_g[l, 2], self.ln_b[l, 2])
        hT = f.sb("hT", [128, 8, 1024], BF16); bhT = Buf()
        actT = f.sb("actT", [128, 22, 1024], BF16); bact = Buf()
        wd = f.sb("wd", [128, 22, D], BF16); bwd = Buf()
        wgu = [f.sb("wgu%d" % i, [128, 8, 256], BF16) for i in range(3)]; bwgu = [Buf() for _ in range(3)]
        facc = f.sb("facc", [128, 8, D], F32); bfacc = [Buf() for _ in range(8)]
        gts = f.sb("gts", [128, 8, NE], F32); bgts = Buf()
        sl = [f.sb("sl%d" % i, [128, 512], F32) for i in range(2)]; bsl = [Buf(), Buf()]
        wi = 0
        si = 0
        for sb in range(T // 1024):
            self.load_hT(hT, bhT, sb, 8)
            if moe:
                f.dma("sp", gts[:], self.GATE[sb * 1024:(sb + 1) * 1024, :].rearrange("(n p) e -> p n e", p=128), reads=[self.bGATE], writes=[bgts])
            for e in range(ne):
                ei = self.ebase[l] + e
                f.dma("sp", wd[:], self.WD[ei], reads=[self.bW[("wd", ei)]], writes=[bwd])
                for c in range(22):
                    w, bw = wgu[wi % 3], bwgu[wi % 3]
                    wi += 1
                    f.dma("sp", w[:], self.WGU[ei, c], reads=[self.bW[("wgu", ei, c)]], writes=[bw])
                    for tb in range(2):
                        ts = slice(tb * 512, (tb + 1) * 512)
                        ba, bg_ = (0, 1) if (si % 2 == 0) else (2, 3)
                        for (bank, c0) in ((ba, 0), (bg_, 128)):
                            for kc in range(8):
                                kw = dict(writes=[self.bpb[bank]]) if kc == 0 else dict(joins=[self.bpb[bank]])
                                f.op("pe", lambda: nc.tensor.matmul(self.pb[bank][:], lhsT=w[:, kc, c0:c0 + 128], rhs=hT[:, kc, ts],
                                                                    start=(kc == 0), stop=(kc == 7)), reads=[bw, bhT], **kw)
                        s_, bs_ = sl[si % 2], bsl[si % 2]
                        si += 1
                        f.op("act", lambda: nc.scalar.activation(out=s_[:], in_=self.pb[ba][:], func=AF.Silu), reads=[self.bpb[ba]], writes=[bs_])
                        kw = dict(writes=[bact]) if (c == 0 and tb == 0) else dict(joins=[bact])
                        f.op("dve", lambda: nc.vector.tensor_tensor(out=actT[:, c, ts], in0=s_[:], in1=self.pb[bg_][:], op=ALU.mult),
                             reads=[bs_, self.bpb[bg_]], **kw)
                for tt in range(8):
                    for hf in range(2):
                        bank = 4 + (tt * 2 + hf) % 2
                        for c in range(22):
                            kw = dict(writes=[self.bpb[bank]]) if c == 0 else dict(joins=[self.bpb[bank]])
                            f.op("pe", lambda: nc.tensor.matmul(self.pb[bank][:], lhsT=actT[:, c, tt * 128:(tt + 1) * 128], rhs=wd[:, c, hf * 512:(hf + 1) * 512],
                                                                start=(c == 0), stop=(c == 21)), reads=[bact, bwd], **kw)
                        fa = facc[:, tt, hf * 512:(hf + 1) * 512]
                        bfa = bfacc[tt]
                        if not moe:
                            kw = dict(writes=[bfa]) if hf == 0 else dict(joins=[bfa])
                            f.op("act", lambda: nc.scalar.copy(out=fa, in_=self.pb[bank][:]), reads=[self.bpb[bank]], **kw)
                        elif e == 0:
                            kw = dict(writes=[bfa]) if hf == 0 else dict(joins=[bfa])
                            f.op("dve", lambda: nc.vector.tensor_scalar(out=fa, in0=self.pb[bank][:], scalar1=gts[:, tt, e:e + 1], scalar2=None, op0=ALU.mult),
                                 reads=[self.bpb[bank], bgts], **kw)
                        else:
                            f.op("dve", lambda: nc.vector.scalar_tensor_tensor(out=fa, in0=self.pb[bank][:], scalar=gts[:, tt, e:e + 1], in1=fa,
                                                                                op0=ALU.mult, op1=ALU.add), reads=[self.bpb[bank], bgts, bfa], writes=[bfa])
            for tt in range(8):
                self.ln_tile(sb * 8 + tt, [facc[:, tt, 0:512], facc[:, tt, 512:1024]], [bfacc[tt], bfacc[tt]], dst=self.out if final else None)
        f.barrier()
        f.pop()

    def build(self, stop_after=None, cv_mode=3):
        f = self.f
        self.setup_common()
        for l in range(self.nl):
            if cv_mode & 1:
                self.convert_layer_attn(l)
            if cv_mode & 2:
                self.convert_layer_ffn(l)
        self.bout = Buf("out")

        def dump():
            for gt in range(self.NT):
                rows = slice(gt * 128, (gt + 1) * 128)
                f.dma("sp", self.out[rows, :], self.H[rows, :], reads=[self.bHt[gt]], joins=[self.bout])
        done = False
        self.phase_embed()
        if stop_after == "embed":
            dump(); done = True
        for l in range(self.nl):
            if done:
                break
            for ph, fn in (("A", self.phase_A), ("B", self.phase_B), ("C", self.phase_C), ("D", self.phase_D)):
                fn(l)
                if stop_after == ph and l == self.nl - 1:
                    dump(); done = True
                    break
            if done:
                break
            self.phase_E(l, final=(l == self.nl - 1))
        deps = {}
        _merge(deps, self.bout.w)
        f._wait("sp", deps)
        f.barrier()
        f.close()
        return self.nc


def _rope_table():
    t = np.arange(S)
    row = (t // 64).astype(np.float32)
    col = (t % 64).astype(np.float32)

    def ang(rot):
        n = rot // 4
        inv = (np.float32(10000.0) ** (-np.arange(n, dtype=np.float32) / np.float32(n))).astype(np.float32)
        return np.concatenate([row[:, None] * inv, col[:, None] * inv], axis=-1).astype(np.float32)
    ab, ac = ang(64), ang(32)
    return np.concatenate([np.cos(ab), np.sin(ab), np.cos(ac), np.sin(ac)], axis=-1).astype(np.float32)


def _na_bias_tables(rpb):
    L = rpb.shape[0]
    outt = np.full((L, 5, 5, 128, 4, 128), NEG, np.float32)
    p = np.arange(128)
    for pi, j in enumerate((0, 1, 2, 30, 31)):
        k0 = min(max(j - 2, 0), 27)
        qr = 2 * j + p // 64
        qc = p % 64
        wr = np.clip(qr - 4, 0, 56)
        wc = np.clip(qc - 8, 0, 48)
        for t in range(5):
            kr = 2 * (k0 + t) + p // 64
            kc = p % 64
            valid = ((kr[:, None] >= wr[None, :]) & (kr[:, None] < wr[None, :] + 8) &
                     (kc[:, None] >= wc[None, :]) & (kc[:, None] < wc[None, :] + 16))
            dr = np.clip(kr[:, None] - qr[None, :] + 7, 0, 14)
            dc = np.clip(kc[:, None] - qc[None, :] + 15, 0, 30)
            g = rpb[:, :, dr, dc]
            g = np.where(valid[None, None], g, np.float32(NEG))
            outt[:, pi, t] = g.transpose(0, 2, 1, 3)
    return outt.reshape(L, 5, 5, 128, 512)


_CACHE = {}


def kernel(**inputs):
    inp = {k: np.ascontiguousarray(np.asarray(v)) for k, v in inputs.items()}
    if "prog" not in _CACHE:
        _CACHE["prog"] = Prog().build()
    nc = _CACHE["prog"]
    rope = _rope_table()
    nab = _na_bias_tables(inp["na_rpb"])
    shared = {k: v for k, v in inp.items() if k not in ("x", "mem", "na_rpb", "emb_ln_g", "emb_ln_b")}
    shared["emb_ln_g"] = inp["emb_ln_g"].reshape(1, D)
    shared["emb_ln_b"] = inp["emb_ln_b"].reshape(1, D)
    shared["rope"] = rope
    shared["moe_router_t"] = np.ascontiguousarray(inp["moe_router"].transpose(0, 2, 1))
    del shared["moe_router"]
    shared["nab"] = nab
    in_maps = []
    for c in range(8):
        b = c % 4
        m = dict(shared)
        m["x"] = inp["x"][b]
        m["mem"] = inp["mem"][b]
        in_maps.append(m)
    res = run_bass_kernel_spmd(nc, in_maps, core_ids=list(range(8)))
    return np.stack([res.results[b]["out"] for b in range(4)], axis=0).astype(np.float32)
```
